# Optimizing a Trainium2 kernel written in Bass

```python
import jax
import jax.numpy as jnp
from jax import lax
import numpy as np

D_MODEL = 1024
BATCH = 8
SEQ = 4096
DEPTH = 4

GRID_W = 64
CTX_LEN = 256
ATT_HEADS = 8
ATT_KV_HEADS = 2
ATT_HEAD_DIM = 64
ATT_GROUP = ATT_HEADS // ATT_KV_HEADS
WINDOW = 128
ATT_BLOCK = 128
ATT_SPAN = ATT_BLOCK + 2 * WINDOW
ROPE_THETA = 10000.0
ML_HEADS = 4
ML_QK_DIM = 64
ML_V_DIM = 128
ML_CHUNK = 64
ML_CONV = 5
GATE_CAP = 15.0
ATT_WIDTH = ATT_HEADS * ATT_HEAD_DIM
ATT_KV_WIDTH = ATT_KV_HEADS * ATT_HEAD_DIM
ML_QK_WIDTH = ML_HEADS * ML_QK_DIM
ML_WIDTH = ML_HEADS * ML_V_DIM
ML_GATES = 4 * ML_HEADS
MIX_WIDTH = ATT_WIDTH + ML_WIDTH
SPLIT_SIZES = (ATT_WIDTH, ATT_KV_WIDTH, ATT_KV_WIDTH, ML_QK_WIDTH, ML_QK_WIDTH, ML_WIDTH, ML_WIDTH, ML_GATES)
IN_WIDTH = ATT_WIDTH + 2 * ATT_KV_WIDTH + 2 * ML_QK_WIDTH + 2 * ML_WIDTH + ML_GATES
FFN_DIM = 2816
N_EXPERTS = 8
TOP_K = 2
EPS = 1e-6

kernel_name = 'hybrid_swa_mlstm_moe_diffusion'


def rmsnorm(x, g):
    xf = x.astype(jnp.float32)
    y = xf * lax.rsqrt(jnp.mean(xf * xf, axis=-1, keepdims=True) + EPS)
    return (y * g.astype(jnp.float32)).astype(x.dtype)


def adaln(x, g, shift, scale):
    return rmsnorm(x, g) * (1 + scale) + shift


def split_columns(u):
    points = [int(p) for p in np.cumsum(SPLIT_SIZES)[:-1]]
    return jnp.split(u, points, axis=-1)


def axial_rope_tables(n_tokens):
    rows = n_tokens // GRID_W
    t = jnp.arange(rows * GRID_W)
    row = jnp.repeat(jnp.arange(rows), GRID_W).astype(jnp.float32)
    col = (t % GRID_W).astype(jnp.float32)
    quarter = ATT_HEAD_DIM // 4
    inv = ROPE_THETA ** (-jnp.arange(quarter, dtype=jnp.float32) / quarter)
    ang_r = row[:, None] * inv[None, :]
    ang_c = col[:, None] * inv[None, :]
    return (jnp.cos(ang_r), jnp.sin(ang_r), jnp.cos(ang_c), jnp.sin(ang_c))


def _rotate_half(x, cos, sin):
    x1, x2 = jnp.split(x, 2, axis=-1)
    return jnp.concatenate([x1 * cos - x2 * sin, x2 * cos + x1 * sin], axis=-1)


def apply_axial_rope(x, tabs):
    cos_r, sin_r, cos_c, sin_c = [t[:, None, :] for t in tabs]
    xf = x.astype(jnp.float32)
    half = x.shape[-1] // 2
    out = jnp.concatenate([_rotate_half(xf[..., :half], cos_r, sin_r),
                           _rotate_half(xf[..., half:], cos_c, sin_c)], axis=-1)
    return out.astype(x.dtype)


def sink_attention(qg, kv_sets, sink_g):
    sink = sink_g[None, :, :, None]
    m = sink
    scores = []
    for k, v, mask in kv_sets:
        s = jnp.einsum('bqhgd,bkhd->bhgqk', qg, k).astype(jnp.float32)
        if mask is not None:
            s = jnp.where(mask, s, -jnp.inf)
        scores.append(s)
        m = jnp.maximum(m, s.max(-1))
    denom = jnp.exp(sink - m)
    num = None
    for s, (k, v, mask) in zip(scores, kv_sets):
        p = jnp.exp(s - m[..., None])
        denom = denom + p.sum(-1)
        o = jnp.einsum('bhgqk,bkhd->bhgqd', p, v.astype(jnp.float32))
        num = o if num is None else num + o
    out = num / denom[..., None]
    return jnp.transpose(out, (0, 3, 1, 2, 4))


def window_attention(q, k, v, k_ctx, v_ctx, sink_g):
    B, S = q.shape[:2]
    nb = S // ATT_BLOCK
    qg = (q * (ATT_HEAD_DIM ** -0.5)).reshape(B, nb, ATT_BLOCK, ATT_KV_HEADS, ATT_GROUP, ATT_HEAD_DIM)
    pad = ((0, 0), (WINDOW, WINDOW), (0, 0), (0, 0))
    kp = jnp.pad(k, pad)
    vp = jnp.pad(v, pad)
    valid = jnp.pad(jnp.ones((S,), dtype=bool), (WINDOW, WINDOW))
    rel = jnp.abs(jnp.arange(ATT_BLOCK)[:, None] + WINDOW - jnp.arange(ATT_SPAN)[None, :]) <= WINDOW

    def block(j):
        start = j * ATT_BLOCK
        qb = lax.dynamic_index_in_dim(qg, j, axis=1, keepdims=False)
        kb = lax.dynamic_slice_in_dim(kp, start, ATT_SPAN, axis=1)
        vb = lax.dynamic_slice_in_dim(vp, start, ATT_SPAN, axis=1)
        mask = rel & lax.dynamic_slice_in_dim(valid, start, ATT_SPAN)[None, :]
        out = sink_attention(qb, ((kb, vb, mask), (k_ctx, v_ctx, None)), sink_g)
        return out.reshape(B, ATT_BLOCK, ATT_WIDTH).astype(q.dtype)

    outs = lax.map(block, jnp.arange(nb))
    return jnp.moveaxis(outs, 0, 1).reshape(B, S, ATT_WIDTH)


def mlstm_chunkwise(q, k, v, i_pre, f_pre, state):
    B, T, H, DK = q.shape
    DV = v.shape[-1]
    nc = T // ML_CHUNK
    f32 = jnp.float32

    def chunks(a):
        return jnp.moveaxis(a.reshape((B, nc, ML_CHUNK) + a.shape[2:]), 1, 0)

    qs = chunks(q.astype(f32) * (DK ** -0.5))
    ks = chunks(k.astype(f32))
    vs = chunks(v.astype(f32))
    lis = chunks(i_pre.astype(f32))
    lfs = chunks(jax.nn.log_sigmoid(f_pre.astype(f32)))
    causal = jnp.tril(jnp.ones((ML_CHUNK, ML_CHUNK), dtype=bool))

    def step(carry, inp):
        C, n, m = carry
        qc, kc, vc, li, lf = inp
        b = jnp.cumsum(lf, axis=1).transpose(0, 2, 1)
        ig = li.transpose(0, 2, 1)
        d = jnp.where(causal, b[..., :, None] - b[..., None, :] + ig[..., None, :], -jnp.inf)
        inter = b + m[..., None]
        m_t = jnp.maximum(inter, d.max(-1))
        w_inter = jnp.exp(inter - m_t)
        s = jnp.einsum('bthd,bshd->bhts', qc, kc) * jnp.exp(d - m_t[..., None])
        num = jnp.einsum('bhts,bshe->bhte', s, vc) + w_inter[..., None] * jnp.einsum('bthd,bhde->bhte', qc, C)
        den = s.sum(-1) + w_inter * jnp.einsum('bthd,bhd->bht', qc, n)
        h = num / jnp.maximum(jnp.abs(den), jnp.exp(-m_t))[..., None]
        b_end = b[..., -1]
        g = b_end[..., None] - b + ig
        m_new = jnp.maximum(b_end + m, g.max(-1))
        decay = jnp.exp(b_end + m - m_new)
        wk = jnp.exp(g - m_new[..., None])
        C_new = decay[..., None, None] * C + jnp.einsum('bhs,bshd,bshe->bhde', wk, kc, vc)
        n_new = decay[..., None] * n + jnp.einsum('bhs,bshd->bhd', wk, kc)
        return (C_new, n_new, m_new), h.transpose(0, 2, 1, 3)

    state, hs = lax.scan(step, state, (qs, ks, vs, lis, lfs))
    h = jnp.moveaxis(hs, 0, 1).reshape(B, T, H, DV)
    return h, state


def depthwise_conv(x, w, b):
    C = x.shape[-1]
    y = lax.conv_general_dilated(x, w.reshape(ML_CONV, 1, C).astype(x.dtype), (1,),
                                 [(ML_CONV // 2, ML_CONV // 2)],
                                 dimension_numbers=('NWC', 'WIO', 'NWC'), feature_group_count=C)
    return y + b


def mlstm_inputs(qm, km, vm, gt, conv_w, conv_b, b_gates):
    B, T, _ = qm.shape
    qk = jax.nn.silu(depthwise_conv(jnp.concatenate([qm, km], axis=-1), conv_w, conv_b))
    q, k = jnp.split(qk, 2, axis=-1)
    q = q.reshape(B, T, ML_HEADS, ML_QK_DIM)
    k = k.reshape(B, T, ML_HEADS, ML_QK_DIM)
    v = vm.reshape(B, T, ML_HEADS, ML_V_DIM)
    g = gt.astype(jnp.float32) + b_gates.astype(jnp.float32)
    g = (GATE_CAP * jnp.tanh(g / GATE_CAP)).reshape(B, T, 4, ML_HEADS)
    return q, k, v, g[:, :, 0], g[:, :, 1], g[:, :, 2], g[:, :, 3]


def init_state(B):
    f32 = jnp.float32
    return (jnp.zeros((B, ML_HEADS, ML_QK_DIM, ML_V_DIM), f32),
            jnp.zeros((B, ML_HEADS, ML_QK_DIM), f32),
            jnp.zeros((B, ML_HEADS), f32))


def merge_groups(att, h_ml, o_pre, g_att, g_ml, w_out):
    B, T = att.shape[:2]
    att_n = rmsnorm(att, g_att)
    hf = h_ml.astype(jnp.float32)
    hn = hf * lax.rsqrt(jnp.mean(hf * hf, axis=-1, keepdims=True) + EPS)
    hn = hn * g_ml.astype(jnp.float32).reshape(ML_HEADS, ML_V_DIM)
    ml = hn.reshape(B, T, ML_WIDTH).astype(att.dtype) * jax.nn.sigmoid(o_pre)
    return jnp.concatenate([att_n, ml], axis=-1) @ w_out


def token_mixing(hx, hc, w_in, conv_w, conv_b, b_gates, attn_sink, g_att, g_ml, w_out, rope, with_ctx_out):
    B, S, _ = hx.shape
    L = hc.shape[1]
    qa_x, ka_x, va_x, qm_x, km_x, vm_x, om_x, gt_x = split_columns(hx @ w_in)
    qa_c, ka_c, va_c, qm_c, km_c, vm_c, om_c, gt_c = split_columns(hc @ w_in)
    sink_g = attn_sink.astype(jnp.float32).reshape(ATT_KV_HEADS, ATT_GROUP)
    k_ctx = ka_c.reshape(B, L, ATT_KV_HEADS, ATT_HEAD_DIM)
    v_ctx = va_c.reshape(B, L, ATT_KV_HEADS, ATT_HEAD_DIM)
    q = apply_axial_rope(qa_x.reshape(B, S, ATT_HEADS, ATT_HEAD_DIM), rope)
    k = apply_axial_rope(ka_x.reshape(B, S, ATT_KV_HEADS, ATT_HEAD_DIM), rope)
    v = va_x.reshape(B, S, ATT_KV_HEADS, ATT_HEAD_DIM)
    att_x = window_attention(q, k, v, k_ctx, v_ctx, sink_g)
    qc_, kc_, vc_, icf, fcf, icb, fcb = mlstm_inputs(qm_c, km_c, vm_c, gt_c, conv_w, conv_b, b_gates)
    qx_, kx_, vx_, ixf, fxf, ixb, fxb = mlstm_inputs(qm_x, km_x, vm_x, gt_x, conv_w, conv_b, b_gates)
    flip = lambda a: jnp.flip(a, axis=1)
    h_cf, st_f = mlstm_chunkwise(qc_, kc_, vc_, icf, fcf, init_state(B))
    h_cb, st_b = mlstm_chunkwise(flip(qc_), flip(kc_), flip(vc_), flip(icb), flip(fcb), init_state(B))
    h_xf, _ = mlstm_chunkwise(qx_, kx_, vx_, ixf, fxf, st_f)
    h_xb, _ = mlstm_chunkwise(flip(qx_), flip(kx_), flip(vx_), flip(ixb), flip(fxb), st_b)
    out_x = merge_groups(att_x, h_xf + flip(h_xb), om_x, g_att, g_ml, w_out)
    if not with_ctx_out:
        return out_x, None
    qg_c = (qa_c * (ATT_HEAD_DIM ** -0.5)).reshape(B, L, ATT_KV_HEADS, ATT_GROUP, ATT_HEAD_DIM)
    att_c = sink_attention(qg_c, ((k_ctx, v_ctx, None),), sink_g).reshape(B, L, ATT_WIDTH).astype(hc.dtype)
    out_c = merge_groups(att_c, h_cf + flip(h_cb), om_c, g_att, g_ml, w_out)
    return out_x, out_c


def swiglu(t, w1, w3, w2):
    return (jax.nn.silu(t @ w1) * (t @ w3)) @ w2


def moe_swiglu(h, w_router, b_router, w1, w3, w2):
    shp = h.shape
    t = h.reshape(-1, shp[-1])
    logits = (t @ w_router).astype(jnp.float32) + b_router.astype(jnp.float32)
    top_v, top_i = lax.top_k(logits, TOP_K)
    top_w = jax.nn.softmax(top_v, axis=-1)
    combine = jnp.sum(jax.nn.one_hot(top_i, N_EXPERTS, dtype=jnp.float32) * top_w[..., None], axis=1)
    y = None
    for e in range(N_EXPERTS):
        ye = combine[:, e:e + 1] * swiglu(t, w1[e], w3[e], w2[e]).astype(jnp.float32)
        y = ye if y is None else y + ye
    return y.astype(h.dtype).reshape(shp)


def channel_mix(h, layer, ffn_w1, ffn_w3, ffn_w2, w_router, b_router, exp_w1, exp_w3, exp_w2):
    i = layer // 2
    if layer % 2 == 0:
        return swiglu(h, ffn_w1[i], ffn_w3[i], ffn_w2[i])
    return moe_swiglu(h, w_router[i], b_router[i], exp_w1[i], exp_w3[i], exp_w2[i])


def setup_inputs(seed: int = 0) -> dict:
    key = jax.random.key(seed)
    ks = jax.random.split(key, 26)
    D = D_MODEL
    n_dense = (DEPTH + 1) // 2
    n_moe = DEPTH // 2

    def nrm(i, shape, scale):
        return jax.random.normal(ks[i], shape, jnp.float32) * scale

    i_bias = -2.0 + nrm(11, (DEPTH, 2, ML_HEADS), 0.1)
    f_bias = 4.0 + nrm(12, (DEPTH, 2, ML_HEADS), 0.5)
    b_gates = jnp.stack([i_bias[:, 0], f_bias[:, 0], i_bias[:, 1], f_bias[:, 1]], axis=1).reshape(DEPTH, ML_GATES)
    return {
        'x': nrm(0, (BATCH, SEQ, D), 1.0),
        'c': nrm(1, (BATCH, D), 1.0),
        'ctx': nrm(2, (BATCH, CTX_LEN, D), 1.0),
        'c_ctx': nrm(3, (D,), 1.0),
        'norm1_g': 1.0 + nrm(4, (DEPTH, D), 0.02),
        'norm2_g': 1.0 + nrm(5, (DEPTH, D), 0.02),
        'w_mod': nrm(6, (DEPTH, D, 6 * D), 0.5 * D ** -0.5),
        'b_mod': nrm(7, (DEPTH, 6 * D), 0.02),
        'w_in': nrm(8, (DEPTH, D, IN_WIDTH), D ** -0.5),
        'conv_w': nrm(9, (DEPTH, ML_CONV, 2 * ML_QK_WIDTH), ML_CONV ** -0.5),
        'conv_b': nrm(10, (DEPTH, 2 * ML_QK_WIDTH), 0.02),
        'b_gates': b_gates,
        'attn_sink': nrm(13, (DEPTH, ATT_HEADS), 0.5),
        'g_att': 1.0 + nrm(14, (DEPTH, ATT_WIDTH), 0.02),
        'g_ml': 1.0 + nrm(15, (DEPTH, ML_WIDTH), 0.02),
        'w_out': nrm(16, (DEPTH, MIX_WIDTH, D), MIX_WIDTH ** -0.5),
        'ffn_w1': nrm(17, (n_dense, D, FFN_DIM), D ** -0.5),
        'ffn_w3': nrm(18, (n_dense, D, FFN_DIM), D ** -0.5),
        'ffn_w2': nrm(19, (n_dense, FFN_DIM, D), FFN_DIM ** -0.5),
        'w_router': nrm(20, (n_moe, D, N_EXPERTS), D ** -0.5),
        'b_router': nrm(21, (n_moe, N_EXPERTS), 0.01),
        'exp_w1': nrm(22, (n_moe, N_EXPERTS, D, FFN_DIM), D ** -0.5),
        'exp_w3': nrm(23, (n_moe, N_EXPERTS, D, FFN_DIM), D ** -0.5),
        'exp_w2': nrm(24, (n_moe, N_EXPERTS, FFN_DIM, D), FFN_DIM ** -0.5),
        'final_g': 1.0 + nrm(25, (D,), 0.02),
    }


def reference(x, c, ctx, c_ctx, norm1_g, norm2_g, w_mod, b_mod, w_in, conv_w, conv_b, b_gates,
              attn_sink, g_att, g_ml, w_out, ffn_w1, ffn_w3, ffn_w2, w_router, b_router,
              exp_w1, exp_w3, exp_w2, final_g):
    rope = axial_rope_tables(x.shape[1])
    cond_x = jax.nn.silu(c)
    cond_c = jax.nn.silu(c_ctx)[None]
    for layer in range(DEPTH):
        last = layer == DEPTH - 1
        mod_x = jnp.split((cond_x @ w_mod[layer] + b_mod[layer])[:, None, :], 6, axis=-1)
        mod_c = jnp.split((cond_c @ w_mod[layer] + b_mod[layer])[:, None, :], 6, axis=-1)
        hx = adaln(x, norm1_g[layer], mod_x[0], mod_x[1])
        hc = adaln(ctx, norm1_g[layer], mod_c[0], mod_c[1])
        mix_x, mix_c = token_mixing(hx, hc, w_in[layer], conv_w[layer], conv_b[layer], b_gates[layer],
                                    attn_sink[layer], g_att[layer], g_ml[layer], w_out[layer], rope,
                                    not last)
        x = x + mod_x[2] * mix_x
        hx = adaln(x, norm2_g[layer], mod_x[3], mod_x[4])
        x = x + mod_x[5] * channel_mix(hx, layer, ffn_w1, ffn_w3, ffn_w2, w_router, b_router,
                                       exp_w1, exp_w3, exp_w2)
        if not last:
            ctx = ctx + mod_c[2] * mix_c
            hc = adaln(ctx, norm2_g[layer], mod_c[3], mod_c[4])
            ctx = ctx + mod_c[5] * channel_mix(hc, layer, ffn_w1, ffn_w3, ffn_w2, w_router, b_router,
                                               exp_w1, exp_w3, exp_w2)
    return rmsnorm(x, final_g)
```

```python
import contextlib
import numpy as np
import concourse.bass as bass
import concourse.mybir as mybir
from concourse.bass_utils import run_bass_kernel_spmd

F32 = mybir.dt.float32
BF16 = mybir.dt.bfloat16
AF = mybir.ActivationFunctionType
ALU = mybir.AluOpType
AX = mybir.AxisListType

D = 1024
CTX = 256
FFN = 2816
NEXP = 8
WIN = 3216
RW = 1056
EPS = 1e-6
NEG = -30000.0
LN8 = float(np.log(0.125))


class Res:
    __slots__ = ("name", "w", "r")

    def __init__(self, name):
        self.name = name
        self.w = None
        self.r = {}


class TT:
    def __init__(self, name, t):
        self.t = t
        self.res = Res(name)

    def __getitem__(self, k):
        return self.t[k]


def _res(x):
    return x.res if isinstance(x, TT) else x


def _freeze(fn):
    import types
    if fn.__closure__ is None:
        return fn
    cells = []
    for c in fn.__closure__:
        try:
            cells.append(types.CellType(c.cell_contents))
        except ValueError:
            cells.append(c)
    return types.FunctionType(fn.__code__, fn.__globals__, fn.__name__, fn.__defaults__, tuple(cells))


class Sched:
    ENGS = ("pe", "act", "dve", "pool", "sp")

    def __init__(self, nc, n_dma_sems=14):
        self.nc = nc
        self.sems = {}
        self.prog = {e: [] for e in self.ENGS}
        self.cnt = {e: 0 for e in self.ENGS}
        self.seen = {e: {} for e in self.ENGS}
        self.pending = {e: False for e in self.ENGS}
        self.nd = n_dma_sems
        self.dma_i = {e: 0 for e in self.ENGS}
        self.dma_val = {}
        self._ctx = []
        for e in self.ENGS:
            self._mksem("E_" + e)
        for q in ("sp", "pool"):
            for i in range(n_dma_sems):
                k = "D_%s_%d" % (q, i)
                self._mksem(k)
                self.dma_val[k] = 0

    def _mksem(self, key):
        cm = self.nc.semaphore(key)
        self.sems[key] = cm.__enter__()
        self._ctx.append(cm)

    def close(self):
        for cm in reversed(self._ctx):
            cm.__exit__(None, None, None)

    def _deps(self, eng, reads, writes):
        need = {}

        def add(tok, kind):
            if tok is None:
                return
            key, val, ename = tok
            if ename == eng and not key.startswith("D_"):
                if eng == "pe" or kind == "war":
                    return
            if need.get(key, 0) < val:
                need[key] = val

        for r in reads:
            add(_res(r).w, "raw")
        for w in writes:
            w = _res(w)
            add(w.w, "waw")
            for key, (val, ename) in w.r.items():
                add((key, val, ename), "war")
        out = []
        for key, val in need.items():
            if self.seen[eng].get(key, 0) >= val:
                continue
            self.seen[eng][key] = val
            out.append((key, val))
        return out

    def _record(self, tok, reads, writes):
        key, val, ename = tok
        for r in reads:
            _res(r).r[key] = (val, ename)
        for w in writes:
            w = _res(w)
            w.w = tok
            w.r = {}

    def op(self, eng, fn, reads=(), writes=(), signal=True):
        fn = _freeze(fn)
        if eng != "pe":
            signal = True
        waits = self._deps(eng, reads, writes)
        key = "E_" + eng
        val = self.cnt[eng] + 1
        sems = self.sems
        if signal:
            self.cnt[eng] = val
        self.pending[eng] = not signal

        def emit(h):
            for k, v in waits:
                h.wait_ge(sems[k], v)
            ins = fn(h)
            if signal:
                ins.then_inc(sems[key], 1)

        self.prog[eng].append(emit)
        self._record((key, val, eng), reads, writes)

    def dma(self, q, out, in_, reads=(), writes=(), **kw):
        i = self.dma_i[q] % self.nd
        self.dma_i[q] += 1
        dkey = "D_%s_%d" % (q, i)
        prev = self.dma_val[dkey]
        waits = self._deps(q, reads, writes)
        if prev > 0 and self.seen[q].get(dkey, 0) < prev:
            self.seen[q][dkey] = prev
            waits.append((dkey, prev))
        val = prev + 16
        self.dma_val[dkey] = val
        sems = self.sems

        def emit(h):
            for k, v in waits:
                h.wait_ge(sems[k], v)
            h.dma_start(out=out, in_=in_, **kw).then_inc(sems[dkey], 16)

        self.prog[q].append(emit)
        self._record((dkey, val, q), reads, writes)

    def barrier(self):
        tgt = {("E_" + e): self.cnt[e] for e in self.ENGS if self.cnt[e] > 0}
        for k, v in self.dma_val.items():
            if v > 0:
                tgt[k] = v
        sems = self.sems
        for e in self.ENGS:
            assert not self.pending[e]
            ws = []
            for k, v in tgt.items():
                if k == "E_" + e and e == "pe":
                    continue
                if self.seen[e].get(k, 0) < v:
                    self.seen[e][k] = v
                    ws.append((k, v))

            def emit(h, ws=ws):
                for k, v in ws:
                    h.wait_ge(sems[k], v)

            self.prog[e].append(emit)

    def run(self):
        nc = self.nc
        for e in self.ENGS:
            assert not self.pending[e], e
        prog = self.prog
        with nc.Block() as block:
            @block.tensor
            def _(h):
                for f in prog["pe"]:
                    f(h)

            @block.scalar
            def _(h):
                for f in prog["act"]:
                    f(h)

            @block.vector
            def _(h):
                for f in prog["dve"]:
                    f(h)

            @block.gpsimd
            def _(h):
                for f in prog["pool"]:
                    f(h)

            @block.sync
            def _(h):
                for f in prog["sp"]:
                    f(h)


def cust(t, pstart, np_, off, dims):
    base = t[:]
    pstep = base.ap[0][0]
    return bass.AP(t, base.offset + pstart * pstep + off, [[pstep, np_]] + [[s, c] for s, c in dims])


class StopBuild(Exception):
    pass


class Cfg:
    def __init__(self, S=4096, depth=4, dbg=False, stop=99):
        self.stop = stop
        self.S = S
        self.depth = depth
        self.NT = CTX + S
        self.NB = self.NT // 128
        self.tiles = [(0, 256)] + [(CTX + 512 * i, 512) for i in range(S // 512)]
        self.nd = (depth + 1) // 2
        self.nm = depth // 2
        self.NV = 24 + 88 * depth
        self.dbg = dbg


def build(cfg):
    S_, L, NT, NB = cfg.S, cfg.depth, cfg.NT, cfg.NB
    nc = bass.Bass("TRN2", target_bir_lowering=False)

    def din(name, shape, dt=F32):
        return nc.dram_tensor(name, list(shape), dt, kind="ExternalInput").ap()

    x_in = din("x", [S_, D])
    ctx_in = din("ctx", [CTX, D])
    vecs_in = din("vecs", [128, cfg.NV])
    rows_in = din("rows", [L, RW])
    wmod_in = din("w_mod", [L, D, 6 * D])
    win_in = din("w_in", [L, D, WIN])
    wout_in = din("w_out", [L, D, D])
    fw1_in = din("ffn_w1", [cfg.nd, D, FFN])
    fw3_in = din("ffn_w3", [cfg.nd, D, FFN])
    fw2_in = din("ffn_w2", [cfg.nd, FFN, D])
    nm1 = max(cfg.nm, 1)
    wr_in = din("w_router", [nm1, 128, 8, NEXP])
    ew1_in = din("exp_w1", [nm1, NEXP, D, FFN])
    ew3_in = din("exp_w3", [nm1, NEXP, D, FFN])
    ew2_in = din("exp_w2", [nm1, NEXP, FFN, D])
    tabs_in = din("tabs", [4, 128, NT])
    consts_in = din("consts", [128, 768])
    out_d = nc.dram_tensor("out", [S_, D], F32, kind="ExternalOutput").ap()

    def dscr(name, shape, dt=F32):
        return TT(name, nc.dram_tensor(name, list(shape), dt).ap())

    xT_d = dscr("xT_d", [128, 8, NT])
    pcd_d = dscr("pcd_d", [128, 4, NT + 16])
    qT_d = dscr("qT_d", [128, 4, NT], BF16)
    qm_d = dscr("qm_d", [128, 2, NT], BF16)
    km_d = dscr("km_d", [128, 2, NT], BF16)
    kmtok_d = dscr("kmtok_d", [NB, 128, 256], BF16)
    vm_d = dscr("vm_d", [NB, 128, 516], BF16)
    og_d = dscr("og_d", [NB, 128, 512], BF16)
    h2_d = dscr("h2_d", [128, 8, NT], BF16)
    comb_d = dscr("comb_d", [NEXP, NT])
    yacc_d = dscr("yacc_d", [128, 8, NT])

    S = Sched(nc)
    es_all = contextlib.ExitStack()

    _uid = [0]

    def sb(es, name, shape, dt=F32):
        _uid[0] += 1
        nm = "s%d_%s" % (_uid[0], name)
        return TT(nm, es.enter_context(nc.sbuf_tensor(nm, list(shape), dt)))

    def pc_col(g):
        return g + 4 if g < CTX else g + 8

    try:
     with es_all:
      es0 = es_all
      try:
        def chk(k):
            if cfg.stop == k:
                raise StopBuild()
        PD = [es0.enter_context(nc.psum_tensor("pd%d" % i, [128, 1024], F32)) for i in range(4)]
        PR = [Res("pb%d" % i) for i in range(8)]

        def pb(i):
            return PD[i // 2][:, (i % 2) * 512:(i % 2) * 512 + 512]

        vecs = sb(es0, "vecs", [128, cfg.NV])
        consts = sb(es0, "consts", [128, 768])
        cbf = sb(es0, "cbf", [128, 512], BF16)
        mods = sb(es0, "mods", [128, L, 48, 2])
        gsv = sb(es0, "gsv", [128, L, 2, 8, 2])
        condT = sb(es0, "condT", [128, 8, 2])
        S.dma("sp", vecs[:], vecs_in[:, :], writes=[vecs])
        S.dma("sp", consts[:], consts_in[:, :], writes=[consts])
        ident = consts[:, 0:128]
        trif = consts[:, 128:256]
        trib = consts[:, 256:384]
        onesf = consts[:, 640:768]
        S.op("dve", lambda h: h.tensor_copy(cbf[:, 0:128], consts[:, 0:128]), reads=[consts], writes=[cbf])
        S.op("dve", lambda h: h.tensor_copy(cbf[:, 128:384], consts[:, 384:640]), reads=[consts], writes=[cbf])
        S.op("dve", lambda h: h.tensor_copy(cbf[:, 384:512], consts[:, 640:768]), reads=[consts], writes=[cbf])
        identb = cbf[:, 0:128]
        maskAb = cbf[:, 128:256]
        maskBb = cbf[:, 256:384]
        onesb = cbf[:, 384:512]

        with contextlib.ExitStack() as es:
            wm = [sb(es, "wm%d" % i, [128, 8, 1024]) for i in range(2)]
            xin = [sb(es, "xin%d" % i, [128, D]) for i in range(2)]
            xo = [sb(es, "xo%d" % i, [128, 8, 128]) for i in range(2)]
            zt = sb(es, "zt", [128, 4, 16])
            S.op("act", lambda h: h.activation(condT[:, :, 0], vecs[:, 0:8], AF.Silu), reads=[vecs], writes=[condT])
            S.op("act", lambda h: h.activation(condT[:, :, 1], vecs[:, 8:16], AF.Silu), reads=[vecs], writes=[condT])
            pi = 0
            for l in range(L):
                for m in range(6):
                    w = wm[pi % 2]
                    pi += 1
                    S.dma("sp", w[:], wmod_in[l, :, m * 1024:(m + 1) * 1024].rearrange("(k p) n -> p k n", p=128), writes=[w])
                    bk = 6
                    for c in range(8):
                        for k in range(8):
                            S.op("pe", lambda h, w=w, c=c, k=k: h.matmul(PD[3][:, c * 2:c * 2 + 2], w[:, k, c * 128:(c + 1) * 128], condT[:, k, :],
                                                                        start=(k == 0), stop=(k == 7)),
                                 reads=[w, condT], writes=[PR[bk]], signal=(k == 7 and c == 7))
                    bcol = 24 + 88 * l + 16 + m * 8
                    S.op("dve", lambda h, l=l, m=m, bcol=bcol: h.tensor_tensor(
                        mods[:, l, m * 8:(m + 1) * 8, :], PD[3][:, 0:16].rearrange("p (c t) -> p c t", t=2),
                        cust(vecs.t, 0, 128, bcol, [(1, 8), (0, 2)]), ALU.add), reads=[PR[bk], vecs], writes=[mods])
                for j, (gcol, mi) in enumerate(((24 + 88 * l, 1), (24 + 88 * l + 8, 4))):
                    S.op("dve", lambda h, l=l, j=j, gcol=gcol, mi=mi: h.scalar_tensor_tensor(
                        gsv[:, l, j, :, :], mods[:, l, mi * 8:(mi + 1) * 8, :], 1.0,
                        cust(vecs.t, 0, 128, gcol, [(1, 8), (0, 2)]), ALU.add, ALU.mult), reads=[mods, vecs], writes=[gsv])
            S.op("dve", lambda h: h.memset(zt[:], 0.0), writes=[zt])
            for c0 in (0, 260, 264 + S_):
                w_ = 4 if c0 != 264 + S_ else 8
                S.dma("sp", pcd_d[:, :, c0:c0 + w_], zt[:, :, 0:w_], reads=[zt], writes=[pcd_d])
            for b in range(NB):
                g0 = b * 128
                xi = xin[b % 2]
                src = ctx_in[g0:g0 + 128, :] if g0 < CTX else x_in[g0 - CTX:g0 - CTX + 128, :]
                S.dma("sp", xi[:], src, writes=[xi])
                for c in range(8):
                    S.op("pe", lambda h, xi=xi, c=c: h.transpose(PD[c // 4][:, (c % 4) * 128:(c % 4) * 128 + 128], xi[:, c * 128:(c + 1) * 128], ident),
                         reads=[xi, consts], writes=[PR[(c // 4) * 2]], signal=(c % 4 == 3))
                o = xo[b % 2]
                S.op("dve", lambda h, o=o: h.tensor_copy(o[:, 0:4, :], PD[0][:, 0:512].rearrange("p (c n) -> p c n", c=4)), reads=[PR[0]], writes=[o])
                S.op("act", lambda h, o=o: h.activation(o[:, 4:8, :], PD[1][:, 0:512].rearrange("p (c n) -> p c n", c=4), AF.Copy), reads=[PR[2]], writes=[o])
                S.dma("sp", xT_d[:, :, g0:g0 + 128], o[:], reads=[o], writes=[xT_d])
        S.barrier()
        chk(0)

        def adaln(X, n, sq, rstd, tmp, hT, l, which, ci, bank):
            S.op("act", lambda h: h.activation(sq[:, :, 0:n], X[:, :, 0:n], AF.Square), reads=[X], writes=[sq])
            chk(30)
            for k in range(8):
                S.op("pe", lambda h, k=k: h.matmul(pb(bank)[:, 0:n], onesb, sq[:, k, 0:n], start=(k == 0), stop=(k == 7)),
                     reads=[cbf, sq], writes=[PR[bank]], signal=(k == 7))
            chk(31)
            S.op("act", lambda h: h.activation(rstd[:, 0:n], pb(bank)[:, 0:n], AF.Sqrt, scale=1.0 / D, bias=epsb[:, 0:1]), reads=[PR[bank], epsb], writes=[rstd])
            chk(32)
            S.op("dve", lambda h: h.reciprocal(rstd[:, 0:n], rstd[:, 0:n]), reads=[rstd], writes=[rstd])
            S.op("dve", lambda h: h.tensor_tensor(tmp[:, :, 0:n], X[:, :, 0:n], cust(rstd.t, 0, 128, 0, [(0, 8), (1, n)]), ALU.mult),
                 reads=[X, rstd], writes=[tmp])
            chk(33)
            shift_i = 0 if which == 0 else 3
            for k in range(8):
                S.op("dve", lambda h, k=k: h.tensor_scalar(hT[:, k, 0:n], tmp[:, k, 0:n], gsv[:, l, which, k, ci:ci + 1],
                                                           mods[:, l, shift_i * 8 + k, ci:ci + 1], ALU.mult, ALU.add),
                     reads=[tmp, mods, gsv], writes=[hT], signal=(k == 7))

        epsb = sb(es0, "epsb", [128, 1])
        S.op("dve", lambda h: h.memset(epsb[:], EPS), writes=[epsb])
        ln8b = sb(es0, "ln8b", [128, 1])
        S.op("dve", lambda h: h.memset(ln8b[:], LN8), writes=[ln8b])

        for l in range(L):
            vb = 24 + 88 * l
            with contextlib.ExitStack() as esAB:
                KT2 = sb(esAB, "KT2", [128, 2, NT], BF16)
                Vt = sb(esAB, "Vt", [128, NB, 128], BF16)
                Graw = sb(esAB, "Graw", [128, NB, 16])
                rowsb = sb(esAB, "rowsb", [128, RW])
                S.dma("sp", rowsb[:], rows_in[l:l + 1, :].partition_broadcast(128), writes=[rowsb])
                with contextlib.ExitStack() as es:
                    winb = sb(es, "winb", [128, 8, WIN], BF16)
                    for k in range(8):
                        S.dma("pool", winb[:, k, :], win_in[l, k * 128:(k + 1) * 128, :], writes=[winb])
                    Xs = [sb(es, "Xa%d" % i, [128, 8, 512]) for i in range(1)]
                    Ya = sb(es, "Ya", [128, 8, 512])
                    sq = sb(es, "sqa", [128, 8, 512], BF16)
                    rstd = sb(es, "rstda", [128, 512])
                    tmp = sb(es, "tmpa", [128, 8, 512])
                    hT = sb(es, "hTa", [128, 8, 512], BF16)
                    tb = [sb(es, "tb%d" % i, [128, 4, 512]) for i in range(1)]
                    r1 = sb(es, "r1", [128, 512])
                    r2 = sb(es, "r2", [128, 512])
                    qst = [sb(es, "qst%d" % i, [128, 4, 512], BF16) for i in range(2)]
                    pcs = [sb(es, "pcs%d" % i, [128, 4, 512]) for i in range(1)]
                    vms = [sb(es, "vms%d" % i, [128, 4, 129], BF16) for i in range(2)]
                    ogs = [sb(es, "ogs%d" % i, [128, 512], BF16) for i in range(2)]
                    sg = sb(es, "sg", [128, 512])
                    for i in range(2):
                        S.op("dve", lambda h, i=i: h.memset(vms[i][:, :, 128:129], 1.0), writes=[vms[i]])
                    chk(20)
                    for ti, (t0, n) in enumerate(cfg.tiles):
                        ci = 1 if t0 < CTX else 0
                        X = Xs[0]
                        S.dma("sp", X[:, :, 0:n], xT_d[:, :, t0:t0 + n], reads=[xT_d], writes=[X])
                        if l > 0:
                            S.dma("sp", Ya[:, :, 0:n], yacc_d[:, :, t0:t0 + n], reads=[yacc_d], writes=[Ya])
                            for c in range(8):
                                S.op("dve", lambda h, c=c: h.scalar_tensor_tensor(X[:, c, 0:n], Ya[:, c, 0:n], mods[:, l - 1, 40 + c, ci:ci + 1], X[:, c, 0:n], ALU.mult, ALU.add),
                                     reads=[Ya, mods, X], writes=[X])
                            S.dma("sp", xT_d[:, :, t0:t0 + n], X[:, :, 0:n], reads=[X], writes=[xT_d])
                        tbt = tb[0]
                        S.dma("sp", tbt[:, :, 0:n], tabs_in[:, :, t0:t0 + n].rearrange("f p n -> p f n"), writes=[tbt])
                        chk(21)
                        adaln(X, n, sq, rstd, tmp, hT, l, 0, ci, 0)
                        chk(10)
                        qs_ = qst[ti % 2]
                        pc = pcs[0]

                        def proj(chunk, bank):
                            for k in range(8):
                                S.op("pe", lambda h, k=k: h.matmul(pb(bank)[:, 0:n], winb[:, k, chunk * 128:(chunk + 1) * 128], hT[:, k, 0:n],
                                                                   start=(k == 0), stop=(k == 7)),
                                     reads=[winb, hT], writes=[PR[bank]], signal=(k == 7))

                        for qi in range(6):
                            ca = qi if qi < 4 else 8 + (qi - 4)
                            cp = 4 + qi if qi < 4 else 10 + (qi - 4)
                            ba, bp = 2 + (qi % 2) * 2, 3 + (qi % 2) * 2
                            proj(ca, ba)
                            proj(cp, bp)
                            tcs = (0, 1) if qi < 4 else (2, 3)
                            S.op("dve", lambda h, ba=ba, tcs=tcs: h.tensor_tensor(r1[:, 0:n], pb(ba)[:, 0:n], tbt[:, tcs[0], 0:n], ALU.mult),
                                 reads=[PR[ba], tbt], writes=[r1])
                            S.op("dve", lambda h, bp=bp, tcs=tcs: h.tensor_tensor(r2[:, 0:n], pb(bp)[:, 0:n], tbt[:, tcs[1], 0:n], ALU.mult),
                                 reads=[PR[bp], tbt], writes=[r2])
                            if qi < 4:
                                S.op("pool", lambda h, qi=qi: h.tensor_tensor(qs_[:, qi, 0:n], r1[:, 0:n], r2[:, 0:n], ALU.add),
                                     reads=[r1, r2], writes=[qs_])
                            else:
                                S.op("pool", lambda h, qi=qi: h.tensor_tensor(KT2[:, qi - 4, t0:t0 + n], r1[:, 0:n], r2[:, 0:n], ALU.add),
                                     reads=[r1, r2], writes=[KT2])
                        S.dma("sp", qT_d[:, :, t0:t0 + n], qs_[:, :, 0:n], reads=[qs_], writes=[qT_d])
                        chk(11)
                        for mi in range(4):
                            bk = 2 + (mi % 2)
                            proj(12 + mi, bk)
                            if mi % 2 == 0:
                                S.op("dve", lambda h, mi=mi, bk=bk: h.tensor_copy(pc[:, mi, 0:n], pb(bk)[:, 0:n]), reads=[PR[bk]], writes=[pc])
                            else:
                                S.op("act", lambda h, mi=mi, bk=bk: h.activation(pc[:, mi, 0:n], pb(bk)[:, 0:n], AF.Copy), reads=[PR[bk]], writes=[pc])
                        c0 = pc_col(t0)
                        S.dma("sp", pcd_d[:, :, c0:c0 + n], pc[:, :, 0:n], reads=[pc], writes=[pcd_d])
                        chk(12)
                        for bi in range(n // 128):
                            blk = (t0 + bi * 128) // 128
                            for gi, (c_lo, c_n, bk) in enumerate(((2048, 128, 6), (2176, 512, 4), (2688, 512, 5), (3200, 16, 7))):
                                for k in range(8):
                                    S.op("pe", lambda h, k=k, c_lo=c_lo, c_n=c_n, bk=bk: h.matmul(
                                        pb(bk)[:, 0:c_n], hT[:, k, bi * 128:(bi + 1) * 128], winb[:, k, c_lo:c_lo + c_n], start=(k == 0), stop=(k == 7)),
                                         reads=[hT, winb], writes=[PR[bk]], signal=(k == 7))
                            S.op("act", lambda h, blk=blk: h.activation(Vt[:, blk, :], pb(6)[:, 0:128], AF.Copy), reads=[PR[6]], writes=[Vt])
                            vmt = vms[blk % 2]
                            S.op("dve", lambda h, vmt=vmt: h.tensor_copy(vmt[:, :, 0:128], pb(4)[:, 0:512].rearrange("p (a e) -> p a e", a=4)),
                                 reads=[PR[4]], writes=[vmt])
                            S.dma("sp", vm_d[blk, :, :], vmt[:].rearrange("p a e -> p (a e)"), reads=[vmt], writes=[vm_d])
                            S.op("act", lambda h: h.activation(sg[:], pb(5)[:, 0:512], AF.Sigmoid), reads=[PR[5]], writes=[sg])
                            ogt = ogs[blk % 2]
                            S.op("dve", lambda h, ogt=ogt: h.tensor_tensor(ogt[:], sg[:], rowsb[:, 536:1048], ALU.mult), reads=[sg, rowsb], writes=[ogt])
                            S.dma("sp", og_d[blk, :, :], ogt[:], reads=[ogt], writes=[og_d])
                            S.op("dve", lambda h, blk=blk: h.tensor_tensor(Graw[:, blk, :], pb(7)[:, 0:16], rowsb[:, 0:16], ALU.add),
                                 reads=[PR[7], rowsb], writes=[Graw])
                S.barrier()
                chk(1)
                with contextlib.ExitStack() as es:
                    win_ = [sb(es, "cw%d" % i, [128, 4, 516]) for i in range(2)]
                    acc = sb(es, "cacc", [128, 4, 512])
                    qk = [sb(es, "cqk%d" % i, [128, 4, 512], BF16) for i in range(2)]
                    ktk = [sb(es, "ktk%d" % i, [128, 4, 256], BF16) for i in range(2)]
                    for ti, (t0, n) in enumerate(cfg.tiles):
                        wv = win_[ti % 2]
                        c0 = pc_col(t0)
                        S.dma("sp", wv[:, :, 0:n + 4], pcd_d[:, :, c0 - 2:c0 + n + 2], reads=[pcd_d], writes=[wv])
                        q_ = qk[ti % 2]
                        for c in range(4):
                            S.op("dve", lambda h, c=c: h.tensor_scalar(acc[:, c, 0:n], wv[:, c, 0:n], vecs[:, vb + 64 + c:vb + 65 + c],
                                                                       vecs[:, vb + 84 + c:vb + 85 + c], ALU.mult, ALU.add),
                                 reads=[wv, vecs], writes=[acc])
                            for j in range(1, 5):
                                S.op("dve", lambda h, c=c, j=j: h.scalar_tensor_tensor(acc[:, c, 0:n], wv[:, c, j:j + n],
                                                                                       vecs[:, vb + 64 + j * 4 + c:vb + 65 + j * 4 + c],
                                                                                       acc[:, c, 0:n], ALU.mult, ALU.add),
                                     reads=[wv, vecs, acc], writes=[acc])
                        S.op("act", lambda h: h.activation(q_[:, :, 0:n], acc[:, :, 0:n], AF.Silu), reads=[acc], writes=[q_])
                        S.dma("sp", qm_d[:, :, t0:t0 + n], q_[:, 0:2, 0:n], reads=[q_], writes=[qm_d])
                        S.dma("sp", km_d[:, :, t0:t0 + n], q_[:, 2:4, 0:n], reads=[q_], writes=[km_d])
                        kt_ = ktk[ti % 2]
                        nb_ = n // 128
                        PTb = PD[0][:, 0:512].bitcast(BF16)
                        for bi in range(nb_):
                            for c in range(2):
                                S.op("pe", lambda h, bi=bi, c=c: h.transpose(PTb[:, (bi * 2 + c) * 128:(bi * 2 + c + 1) * 128],
                                                                             q_[:, 2 + c, bi * 128:(bi + 1) * 128], identb),
                                     reads=[q_, cbf], writes=[PR[0]], signal=(bi == nb_ - 1 and c == 1))
                        S.op("dve", lambda h, nb_=nb_: h.tensor_copy(kt_[:, 0:nb_, :], PTb[:, 0:nb_ * 256].rearrange("p (b c) -> p b c", c=256)),
                             reads=[PR[0]], writes=[kt_])
                        blk0 = t0 // 128
                        S.dma("sp", kmtok_d[blk0:blk0 + nb_, :, :].rearrange("b p c -> p b c"), kt_[:, 0:nb_, :], reads=[kt_], writes=[kmtok_d])
                S.barrier()
                chk(2)

                with contextlib.ExitStack() as es:
                    woutb = sb(es, "woutb", [128, 8, D], BF16)
                    for k in range(8):
                        S.dma("pool", woutb[:, k, :], wout_in[l, k * 128:(k + 1) * 128, :], writes=[woutb])
                    G = sb(es, "G", [128, NB, 16])
                    LF = sb(es, "LF", [128, NB, 2, 4])
                    LI = sb(es, "LI", [128, NB, 2, 4])
                    Bc = sb(es, "Bc", [128, NB, 8])
                    Aa = sb(es, "Aa", [128, NB, 8])
                    WK = sb(es, "WK", [128, NB, 8])
                    DEC = sb(es, "DEC", [128, NB, 2, 2])
                    BT2 = sb(es, "BT2", [128, NB, 8])
                    S.op("act", lambda h: h.activation(G[:], Graw[:], AF.Tanh, scale=1.0 / 15.0), reads=[Graw], writes=[G])
                    S.op("dve", lambda h: h.tensor_scalar(G[:], G[:], 15.0, None, ALU.mult), reads=[G], writes=[G])
                    Gv = G[:].rearrange("p b (k a) -> p b k a", k=4)
                    for d_ in range(2):
                        S.op("dve", lambda h, d_=d_: h.tensor_copy(LI[:, :, d_, :], Gv[:, :, 2 * d_, :]), reads=[G], writes=[LI])
                        S.op("act", lambda h, d_=d_: h.activation(LF[:, :, d_, :], Gv[:, :, 2 * d_ + 1, :], AF.Exp, scale=-1.0), reads=[G], writes=[LF])
                    S.op("act", lambda h: h.activation(LF[:], LF[:], AF.Ln, bias=1.0), reads=[LF], writes=[LF])
                    S.op("dve", lambda h: h.tensor_scalar(LF[:], LF[:], -1.0, None, ALU.mult), reads=[LF], writes=[LF])
                    for b in range(NB):
                        S.op("pe", lambda h, b=b: h.matmul(PD[0][:, 0:4], trif, LF[:, b, 0, :], start=True, stop=True), reads=[consts, LF], writes=[PR[0]], signal=False)
                        S.op("pe", lambda h, b=b: h.matmul(PD[0][:, 4:8], trib, LF[:, b, 1, :], start=True, stop=True), reads=[consts, LF], writes=[PR[0]], signal=False)
                        S.op("pe", lambda h, b=b: h.matmul(PD[0][:, 8:16], onesf, LF[:, b, :, :].rearrange("p a b -> p (a b)"), start=True, stop=True),
                             reads=[consts, LF], writes=[PR[0]])
                        S.op("dve", lambda h, b=b: h.tensor_copy(Bc[:, b, :], PD[0][:, 0:8]), reads=[PR[0]], writes=[Bc])
                        S.op("dve", lambda h, b=b: h.tensor_copy(BT2[:, b, :], PD[0][:, 8:16]), reads=[PR[0]], writes=[BT2])
                    LIf = LI[:].rearrange("p b a c -> p b (a c)")
                    S.op("dve", lambda h: h.tensor_tensor(Aa[:], LIf, Bc[:], ALU.subtract), reads=[LI, Bc], writes=[Aa])
                    S.op("dve", lambda h: h.tensor_tensor(WK[:], Aa[:], BT2[:], ALU.add), reads=[Aa, BT2], writes=[WK])
                    S.op("act", lambda h: h.activation(WK[:], WK[:], AF.Exp), reads=[WK], writes=[WK])
                    S.op("dve", lambda h: h.tensor_scalar(Aa[:], Aa[:], LN8, None, ALU.add), reads=[Aa], writes=[Aa])
                    for half in range(2):
                        S.op("act", lambda h, half=half: h.activation(
                            DEC[half * 64:(half + 1) * 64, :, :, :],
                            cust(BT2.t, half * 64, 64, half, [(8, NB), (4, 2), (2, 2)]), AF.Exp), reads=[BT2], writes=[DEC])

                    CS = sb(es, "CS", [128, NB, 2, 2, 129], BF16)
                    Cst = sb(es, "Cst", [128, 2, 2, 129])
                    S.op("dve", lambda h: h.memset(Cst[:], 0.0), writes=[Cst])
                    order_f = list(range(NB))
                    order_b = [1, 0] + list(range(NB - 1, 1, -1))
                    kws = [sb(es, "kw%d" % i, [128, 2, 4, 64], BF16) for i in range(2)]
                    ktl = [sb(es, "ktl%d" % i, [128, 2, 256], BF16) for i in range(2)]
                    vml = [sb(es, "vml%d" % i, [128, 2, 516], BF16) for i in range(2)]
                    for i in range(NB):
                        fb, bb = order_f[i], order_b[i]
                        kt_, vm_, kw = ktl[i % 2], vml[i % 2], kws[i % 2]
                        S.dma("sp", kt_[:, 0, :], kmtok_d[fb, :, :], reads=[kmtok_d], writes=[kt_])
                        S.dma("sp", kt_[:, 1, :], kmtok_d[bb, :, :], reads=[kmtok_d], writes=[kt_])
                        S.dma("sp", vm_[:, 0, :], vm_d[fb, :, :], reads=[vm_d], writes=[vm_])
                        S.dma("sp", vm_[:, 1, :], vm_d[bb, :, :], reads=[vm_d], writes=[vm_])
                        S.op("act", lambda h, fb=fb: h.activation(CS[:, fb, 0, :, :], Cst[:, 0, :, :], AF.Copy), reads=[Cst], writes=[CS])
                        S.op("act", lambda h, bb=bb: h.activation(CS[:, bb, 1, :, :], Cst[:, 1, :, :], AF.Copy), reads=[Cst], writes=[CS])
                        for d_, blk in ((0, fb), (1, bb)):
                            S.op("dve", lambda h, d_=d_, blk=blk: h.tensor_tensor(
                                kw[:, d_, :, :], kt_[:, d_, :].rearrange("p (a e) -> p a e", a=4),
                                cust(WK.t, 0, 128, blk * 8 + d_ * 4, [(1, 4), (0, 64)]), ALU.mult), reads=[kt_, WK], writes=[kw])
                        for d_ in range(2):
                            for hd in range(4):
                                bank = 2 + d_
                                S.op("pe", lambda h, d_=d_, hd=hd, bank=bank: h.matmul(
                                    pb(bank)[(hd % 2) * 64:(hd % 2) * 64 + 64, (hd // 2) * 129:(hd // 2) * 129 + 129],
                                    kw[:, d_, hd, :], vm_[:, d_, hd * 129:(hd + 1) * 129], start=True, stop=True),
                                     reads=[kw, vm_], writes=[PR[bank]], signal=(hd == 3))
                        for d_, blk in ((0, fb), (1, bb)):
                            S.op("dve", lambda h, d_=d_, blk=blk: h.tensor_tensor(
                                Cst[:, d_, :, :], Cst[:, d_, :, :], cust(DEC.t, 0, 128, blk * 4 + d_ * 2, [(1, 2), (0, 129)]), ALU.mult),
                                 reads=[Cst, DEC], writes=[Cst])
                            S.op("dve", lambda h, d_=d_: h.tensor_tensor(
                                Cst[:, d_, :, :], Cst[:, d_, :, :], pb(2 + d_)[:, 0:258].rearrange("p (a e) -> p a e", a=2), ALU.add),
                                 reads=[Cst, PR[2 + d_]], writes=[Cst])

                    S.barrier()
                    chk(3)
                    qtl = [sb(es, "qtl%d" % i, [128, 4, 128], BF16) for i in range(2)]
                    qml = [sb(es, "qml%d" % i, [128, 2, 128], BF16) for i in range(2)]
                    kml = [sb(es, "kml%d" % i, [128, 2, 128], BF16) for i in range(2)]
                    vm1 = [sb(es, "vm1%d" % i, [128, 516], BF16) for i in range(2)]
                    ogl = [sb(es, "ogl%d" % i, [128, 512], BF16) for i in range(2)]
                    xbl = [sb(es, "xbl%d" % i, [128, 8, 128]) for i in range(3)]
                    Pm = [sb(es, "Pm%d" % i, [128, 640], BF16) for i in range(2)]
                    PTs = [sb(es, "PTs%d" % i, [128, 5, 128], BF16) for i in range(2)]
                    rmax = sb(es, "rmax", [128, 8])
                    negm = sb(es, "negm", [128, 8])
                    rs = sb(es, "rs", [128, 8])
                    es_ = sb(es, "es_", [128, 8])
                    att = sb(es, "att", [128, 8, 64])
                    ss1 = sb(es, "ss1", [128, 1])
                    junk = sb(es, "junk", [128, 512])
                    cats = [sb(es, "cat%d" % i, [128, D], BF16) for i in range(2)]
                    catT = sb(es, "catT", [128, 8, 128], BF16)
                    Rm = sb(es, "Rm", [128, 8, 128])
                    Em = sb(es, "Em", [128, 8, 128], BF16)
                    Emm = sb(es, "Emm", [128, 8, 128], BF16)
                    PTm = sb(es, "PTm", [128, 8, 128], BF16)
                    EB = sb(es, "EB", [128, 8, 128])
                    QS = sb(es, "QS", [128, 2, 2, 128], BF16)
                    den = sb(es, "den", [128, 2, 4])
                    hsum = sb(es, "hsum", [128, 4, 128])
                    h2 = sb(es, "h2", [128, 4, 128])
                    ss4 = sb(es, "ss4", [128, 4])
                    t512 = sb(es, "t512", [128, 512])

                    def loads(b):
                        g0 = b * 128
                        S.dma("sp", qtl[b % 2][:], qT_d[:, :, g0:g0 + 128], reads=[qT_d], writes=[qtl[b % 2]])
                        S.dma("sp", qml[b % 2][:], qm_d[:, :, g0:g0 + 128], reads=[qm_d], writes=[qml[b % 2]])
                        S.dma("sp", kml[b % 2][:], km_d[:, :, g0:g0 + 128], reads=[km_d], writes=[kml[b % 2]])
                        S.dma("sp", vm1[b % 2][:], vm_d[b, :, :], reads=[vm_d], writes=[vm1[b % 2]])
                        S.dma("sp", ogl[b % 2][:], og_d[b, :, :], reads=[og_d], writes=[ogl[b % 2]])
                        S.dma("sp", xbl[b % 3][:], xT_d[:, :, g0:g0 + 128], reads=[xT_d], writes=[xbl[b % 3]])

                    mg = [None]

                    def merge_gen(b):
                        g0 = b * 128
                        cat_ = cats[b % 2]
                        xb_ = xbl[b % 3]
                        ci = 1 if g0 < CTX else 0
                        PTb = PD[0][:, :].bitcast(BF16)
                        for c in range(8):
                            S.op("pe", lambda h, c=c: h.transpose(PTb[:, c * 128:(c + 1) * 128], cat_[:, c * 128:(c + 1) * 128], identb),
                                 reads=[cat_, cbf], writes=[PR[0]], signal=(c == 7))
                        yield
                        S.op("dve", lambda h: h.tensor_copy(catT[:], PTb[:, 0:1024].rearrange("p (c t) -> p c t", c=8)), reads=[PR[0]], writes=[catT])
                        yield
                        for c in range(8):
                            for k in range(8):
                                S.op("pe", lambda h, c=c, k=k: h.matmul(PD[1][:, c * 128:(c + 1) * 128], woutb[:, k, c * 128:(c + 1) * 128], catT[:, k, :],
                                                                        start=(k == 0), stop=(k == 7), skip_group_check=True),
                                     reads=[woutb, catT], writes=[PR[2 + c // 4]], signal=(k == 7 and c % 4 == 3))
                            if c % 2 == 1:
                                yield
                        for c in range(8):
                            S.op("dve", lambda h, c=c: h.scalar_tensor_tensor(xb_[:, c, :], PD[1][:, c * 128:(c + 1) * 128], mods[:, l, 16 + c, ci:ci + 1],
                                                                              xb_[:, c, :], ALU.mult, ALU.add),
                                 reads=[PR[2 + c // 4], mods, xb_], writes=[xb_])
                            if c % 4 == 3:
                                yield
                        S.dma("sp", xT_d[:, :, g0:g0 + 128], xb_[:], reads=[xb_], writes=[xT_d])

                    def tick():
                        if mg[0] is not None:
                            try:
                                next(mg[0])
                            except StopIteration:
                                mg[0] = None

                    def flush():
                        while mg[0] is not None:
                            tick()

                    loads(0)
                    for b in range(NB):
                        if b + 1 < NB:
                            loads(b + 1)
                        g0 = b * 128
                        if l == L - 1 and g0 < CTX:
                            continue
                        qt, qm_, km_, vmb, og, xb = qtl[b % 2], qml[b % 2], kml[b % 2], vm1[b % 2], ogl[b % 2], xbl[b % 3]
                        cat = cats[b % 2]
                        if g0 < CTX:
                            segs = [(0, CTX, None)]
                        else:
                            j = (g0 - CTX) // 128
                            segs = []
                            if j > 0:
                                segs.append((g0 - 128, 128, "A"))
                            segs.append((g0, 128, None))
                            if j < S_ // 128 - 1:
                                segs.append((g0 + 128, 128, "B"))
                            segs.append((0, CTX, None))
                        nk = sum(s[1] for s in segs)
                        def _hd_vars(hd):
                            kv = hd // 4
                            ph = (hd % 2) * 64
                            pd_i = 2 + (hd % 2)
                            return kv, ph, pd_i, pd_i * 2, pd_i * 2 + 1, PD[pd_i]

                        def att_scores(hd):
                            kv = hd // 4
                            ph = (hd % 2) * 64
                            pd_i = 2 + (hd % 2)
                            bA, bB = pd_i * 2, pd_i * 2 + 1
                            SC = PD[pd_i]
                            col = 0
                            mms = []
                            for (k0, kn, mk) in segs:
                                pieces = []
                                c_, k_, r_ = col, k0, kn
                                while r_ > 0:
                                    w_ = min(r_, 512 - (c_ % 512)) if c_ < 512 else r_
                                    pieces.append((c_, k_, w_))
                                    c_ += w_
                                    k_ += w_
                                    r_ -= w_
                                for (c_, k_, w_) in pieces:
                                    mms.append(("s", c_, k_, w_))
                                if mk is not None:
                                    mms.append(("m", col, mk, 128))
                                col += kn
                            started = set()
                            for mi_, (kind, c_, k_, w_) in enumerate(mms):
                                bank = bA if c_ < 512 else bB
                                st = bank not in started
                                started.add(bank)
                                last = mi_ == len(mms) - 1
                                if kind == "s":
                                    S.op("pe", lambda h, c_=c_, k_=k_, w_=w_, st=st: h.matmul(
                                        SC[:, c_:c_ + w_], qt[ph:ph + 64, hd // 2, :], KT2[ph:ph + 64, kv, k_:k_ + w_], start=st, stop=False, skip_group_check=True),
                                         reads=[qt, KT2], writes=[PR[bank]], signal=last)
                                else:
                                    mt = maskAb if k_ == "A" else maskBb
                                    S.op("pe", lambda h, c_=c_, mt=mt, st=st: h.matmul(SC[:, c_:c_ + 128], identb, mt, start=st, stop=False, skip_group_check=True),
                                         reads=[cbf], writes=[PR[bank]], signal=last)

                        def att_rest(hd):
                            kv, ph, pd_i, bA, bB, SC = _hd_vars(hd)
                            rd = [PR[bA]] + ([PR[bB]] if nk > 512 else [])
                            S.op("dve", lambda h, hd=hd: h.tensor_reduce(rmax[:, hd:hd + 1], SC[:, 0:nk], AX.X, ALU.max), reads=rd, writes=[rmax])
                            S.op("dve", lambda h, hd=hd: h.tensor_scalar(negm[:, hd:hd + 1], rmax[:, hd:hd + 1], rowsb[:, 16 + hd:17 + hd], -1.0, ALU.max, ALU.mult),
                                 reads=[rmax, rowsb], writes=[negm])
                            P_ = Pm[hd % 2]
                            S.op("act", lambda h, hd=hd, P_=P_: h.activation(P_[:, 0:nk], SC[:, 0:nk], AF.Exp, bias=negm[:, hd:hd + 1], accum_out=rs[:, hd:hd + 1]),
                                 reads=rd + [negm], writes=[P_, rs])
                            nkb = nk // 128
                            PTb = PD[0][:, (hd % 2) * 512:(hd % 2) * 512 + 512].bitcast(BF16)
                            for kb in range(nkb):
                                S.op("pe", lambda h, kb=kb, P_=P_: h.transpose(PTb[:, kb * 128:(kb + 1) * 128], P_[:, kb * 128:(kb + 1) * 128], identb),
                                     reads=[P_, cbf], writes=[PR[hd % 2]], signal=(kb == nkb - 1))
                            PT_ = PTs[hd % 2]
                            S.op("act" if hd % 2 else "dve",
                                 (lambda h, PT_=PT_, PTb=PTb, nkb=nkb: h.activation(PT_[:, 0:nkb, :], PTb[:, 0:nkb * 128].rearrange("p (a q) -> p a q", q=128), AF.Copy)) if hd % 2 else
                                 (lambda h, PT_=PT_, PTb=PTb, nkb=nkb: h.tensor_copy(PT_[:, 0:nkb, :], PTb[:, 0:nkb * 128].rearrange("p (a q) -> p a q", q=128))),
                                 reads=[PR[hd % 2]], writes=[PT_])
                            kb = 0
                            for (k0, kn, mk) in segs:
                                for s_ in range(kn // 128):
                                    kblk = (k0 + s_ * 128) // 128
                                    S.op("pe", lambda h, kb=kb, kblk=kblk, hd=hd, kv=kv: h.matmul(
                                        PD[1][:, hd * 64:(hd + 1) * 64], PT_[:, kb, :], Vt[:, kblk, kv * 64:(kv + 1) * 64], start=(kb == 0), stop=(kb == nkb - 1),
                                        skip_group_check=True),
                                         reads=[PT_, Vt], writes=[PR[2]], signal=(kb == nkb - 1))
                                    kb += 1

                        att_scores(0)
                        for hd in range(8):
                            if hd + 1 < 8:
                                att_scores(hd + 1)
                            att_rest(hd)
                        S.op("dve", lambda h: h.tensor_tensor(es_[:], rowsb[:, 16:24], negm[:], ALU.add), reads=[rowsb, negm], writes=[es_])
                        S.op("act", lambda h: h.activation(es_[:], es_[:], AF.Exp), reads=[es_], writes=[es_])
                        S.op("dve", lambda h: h.tensor_tensor(rs[:], rs[:], es_[:], ALU.add), reads=[rs, es_], writes=[rs])
                        S.op("dve", lambda h: h.reciprocal(rs[:], rs[:]), reads=[rs], writes=[rs])
                        S.op("dve", lambda h: h.tensor_tensor(att[:], PD[1][:, 0:512].rearrange("p (a e) -> p a e", a=8),
                                                              cust(rs.t, 0, 128, 0, [(1, 8), (0, 64)]), ALU.mult), reads=[PR[2], rs], writes=[att])
                        attf = att[:].rearrange("p a e -> p (a e)")
                        S.op("act", lambda h: h.activation(junk[:], attf, AF.Square, accum_out=ss1[:]), reads=[att], writes=[junk, ss1])
                        S.op("act", lambda h: h.activation(ss1[:], ss1[:], AF.Sqrt, scale=1.0 / 512, bias=epsb[:, 0:1]), reads=[ss1, epsb], writes=[ss1])
                        S.op("dve", lambda h: h.reciprocal(ss1[:], ss1[:]), reads=[ss1], writes=[ss1])
                        S.op("dve", lambda h: h.scalar_tensor_tensor(cat[:, 0:512], attf, ss1[:, 0:1], rowsb[:, 24:536], ALU.mult, ALU.mult),
                             reads=[att, ss1, rowsb], writes=[cat])

                        chk(40)
                        for hd in (0, 2, 1, 3):
                            ph = (hd % 2) * 64
                            S.op("pe", lambda h, hd=hd, ph=ph: h.matmul(pb(4 + hd % 2)[:, (hd // 2) * 128:(hd // 2 + 1) * 128], km_[ph:ph + 64, hd // 2, :], qm_[ph:ph + 64, hd // 2, :],
                                                                        start=(hd // 2 == 0), stop=(hd // 2 == 1), skip_group_check=True),
                                 reads=[km_, qm_], writes=[PR[4 + hd % 2]], signal=(hd // 2 == 1))
                        tick()
                        S.op("dve", lambda h, b=b: h.tensor_tensor(
                            Rm[:].rearrange("p (d a) t -> p d a t", d=2), cust(consts.t, 0, 128, 128, [(128, 2), (0, 4), (1, 128)]),
                            cust(LF.t, 0, 128, b * 8, [(4, 2), (1, 4), (0, 128)]), ALU.mult), reads=[consts, LF], writes=[Rm])
                        for d_ in range(2):
                            S.op("pe", lambda h, d_=d_: h.matmul(PD[3][:, d_ * 512:(d_ + 1) * 512], onesf, Rm[:, d_ * 4:(d_ + 1) * 4, :].rearrange("p a t -> p (a t)"),
                                                                 start=True, stop=True), reads=[consts, Rm], writes=[PR[6 + d_]])
                        chk(50)
                        tick()
                        for dh in range(8):
                            S.op("act", lambda h, dh=dh, b=b: h.activation(Em[:, dh, :], PD[3][:, dh * 128:(dh + 1) * 128], AF.Exp, bias=Aa[:, b, dh:dh + 1]),
                                 reads=[PR[6 + dh // 4], Aa], writes=[Em], signal=(dh == 7))
                        S.op("act", lambda h: h.activation(EB[:].rearrange("p a t -> p (a t)"), PD[3][:, :], AF.Exp, bias=ln8b[:, 0:1]),
                             reads=[PR[6], PR[7], ln8b], writes=[EB])
                        chk(51)
                        tick()
                        S.op("pool", lambda h: h.affine_select(Emm[:, 0:4, :], Em[:, 0:4, :], [[0, 4], [1, 128]], ALU.is_ge, 0.0, base=0, channel_multiplier=-1),
                             reads=[Em], writes=[Emm])
                        S.op("pool", lambda h: h.affine_select(Emm[:, 4:8, :], Em[:, 4:8, :], [[0, 4], [-1, 128]], ALU.is_ge, 0.0, base=0, channel_multiplier=1),
                             reads=[Em], writes=[Emm])
                        for d_ in range(2):
                            for par in range(2):
                                S.op("dve", lambda h, d_=d_, par=par: h.tensor_tensor(
                                    cust(PTm.t, 0, 128, (d_ * 4 + par) * 128, [(256, 2), (1, 128)]), pb(4 + par)[:, 0:256].rearrange("p (a t) -> p a t", a=2),
                                    cust(Emm.t, 0, 128, (d_ * 4 + par) * 128, [(256, 2), (1, 128)]), ALU.mult), reads=[PR[4 + par], Emm], writes=[PTm])
                        tick()
                        for half in range(2):
                            S.op("dve", lambda h, half=half: h.tensor_tensor(
                                QS[half * 64:(half + 1) * 64, :, :, :], cust(qm_.t, half * 64, 64, 0, [(0, 2), (128, 2), (1, 128)]),
                                cust(EB.t, half * 64, 64, half * 128, [(512, 2), (256, 2), (1, 128)]), ALU.mult), reads=[qm_, EB], writes=[QS])
                        chk(52)
                        tick()
                        for d_ in range(2):
                            for hd in range(4):
                                ph = (hd % 2) * 64
                                bank = 4 + 0
                                o = PD[2][:, 0:1024] if False else None
                                dst = PD[2 if d_ == 0 else 3]
                                col = (hd // 2) * 512 + (hd % 2) * 129
                                bk = (4 if d_ == 0 else 6) + hd // 2
                                S.op("pe", lambda h, d_=d_, hd=hd, dst=dst, col=col: h.matmul(
                                    dst[:, col:col + 129], PTm[:, d_ * 4 + hd, :], vmb[:, hd * 129:(hd + 1) * 129], start=True, stop=False, skip_group_check=True),
                                     reads=[PTm, vmb], writes=[PR[bk]], signal=False)
                                S.op("pe", lambda h, d_=d_, hd=hd, dst=dst, col=col, ph=ph, b=b: h.matmul(
                                    dst[:, col:col + 129], QS[ph:ph + 64, d_, hd // 2, :], CS[ph:ph + 64, b, d_, hd // 2, :], start=False, stop=True, skip_group_check=True),
                                     reads=[QS, CS], writes=[PR[bk]], signal=(hd % 2 == 1))
                        chk(53)
                        tick()
                        for d_ in range(2):
                            src = PD[2 if d_ == 0 else 3]
                            bks = [PR[4], PR[5]] if d_ == 0 else [PR[6], PR[7]]
                            S.op("act", lambda h, d_=d_, src=src: h.activation(
                                den[:, d_, :].rearrange("p (a c) -> p a c", a=2), cust(src, 0, 128, 128, [(512, 2), (129, 2)]), AF.Abs),
                                 reads=bks, writes=[den])
                        S.op("dve", lambda h: h.tensor_scalar(den[:], den[:], 1.0, None, ALU.max), reads=[den], writes=[den])
                        S.op("dve", lambda h: h.reciprocal(den[:], den[:]), reads=[den], writes=[den])
                        for d_ in range(2):
                            src = PD[2 if d_ == 0 else 3]
                            bks = [PR[4], PR[5]] if d_ == 0 else [PR[6], PR[7]]
                            dstt = hsum if d_ == 0 else h2
                            for hh in range(2):
                                S.op("dve", lambda h, d_=d_, hh=hh, src=src, dstt=dstt: h.tensor_tensor(
                                    dstt[:, hh * 2:hh * 2 + 2, :], cust(src, 0, 128, hh * 512, [(129, 2), (1, 128)]),
                                    cust(den.t, 0, 128, d_ * 4 + hh * 2, [(1, 2), (0, 128)]), ALU.mult), reads=[bks[hh], den], writes=[dstt])
                        S.op("pool", lambda h: h.tensor_tensor(hsum[:], hsum[:], h2[:], ALU.add), reads=[hsum, h2], writes=[hsum])
                        tick()
                        S.op("pool", lambda h: h.tensor_tensor(h2[:], hsum[:], hsum[:], ALU.mult), reads=[hsum], writes=[h2])
                        S.op("dve", lambda h: h.tensor_reduce(ss4[:], h2[:], AX.X, ALU.add), reads=[h2], writes=[ss4])
                        S.op("act", lambda h: h.activation(ss4[:], ss4[:], AF.Sqrt, scale=1.0 / 128, bias=epsb[:, 0:1]), reads=[ss4, epsb], writes=[ss4])
                        S.op("dve", lambda h: h.reciprocal(ss4[:], ss4[:]), reads=[ss4], writes=[ss4])
                        S.op("dve", lambda h: h.tensor_tensor(t512[:].rearrange("p (a e) -> p a e", a=4), hsum[:], cust(ss4.t, 0, 128, 0, [(1, 4), (0, 128)]), ALU.mult),
                             reads=[hsum, ss4], writes=[t512])
                        S.op("dve", lambda h: h.tensor_tensor(cat[:, 512:1024], t512[:], og[:], ALU.mult), reads=[t512, og], writes=[cat])
                        chk(41)
                        flush()
                        mg[0] = merge_gen(b)
                    flush()
                S.barrier()
            chk(4)

            moe = (l % 2 == 1)
            li = l // 2
            tiles_c = cfg.tiles[1:] if l == L - 1 else cfg.tiles
            with contextlib.ExitStack() as es:
                Xs = [sb(es, "Xc%d" % i, [128, 8, 512]) for i in range(2)]
                sq = sb(es, "sqc", [128, 8, 512], BF16)
                rstd = sb(es, "rstdc", [128, 512])
                tmp = sb(es, "tmpc", [128, 8, 512])
                hT = sb(es, "hTc", [128, 8, 512], BF16)
                if moe:
                    wrt = sb(es, "wrt", [128, 8, NEXP])
                    S.dma("sp", wrt[:], wr_in[li, :, :, :], writes=[wrt])
                    rowsb2 = sb(es, "rowsb2", [128, NEXP])
                    S.dma("sp", rowsb2[:], rows_in[l:l + 1, 1048:1056].partition_broadcast(128), writes=[rowsb2])
                    lg = sb(es, "lg", [128, NEXP])
                    m1 = sb(es, "m1", [128, 1])
                    m2 = sb(es, "m2", [128, 1])
                    k1 = sb(es, "k1", [128, NEXP])
                    k2 = sb(es, "k2", [128, NEXP])
                    lg2 = sb(es, "lg2", [128, NEXP])
                    w1s = sb(es, "w1s", [128, 1])
                    w2s = sb(es, "w2s", [128, 1])
                    cmb = sb(es, "cmb", [128, NEXP])
                    dg = sb(es, "dg", [128, NEXP, 128])
                    cbt = sb(es, "cbt", [128, NEXP, 128])
                for ti, (t0, n) in enumerate(tiles_c):
                    ci = 1 if t0 < CTX else 0
                    X = Xs[ti % 2]
                    S.dma("sp", X[:, :, 0:n], xT_d[:, :, t0:t0 + n], reads=[xT_d], writes=[X])
                    adaln(X, n, sq, rstd, tmp, hT, l, 1, ci, 0)
                    S.dma("sp", h2_d[:, :, t0:t0 + n], hT[:, :, 0:n], reads=[hT], writes=[h2_d])
                    if moe:
                        for k in range(8):
                            S.op("dve", lambda h, k=k: h.tensor_scalar(tmp[:, k, 0:n], tmp[:, k, 0:n], gsv[:, l, 1, k, ci:ci + 1], mods[:, l, 24 + k, ci:ci + 1], ALU.mult, ALU.add),
                                 reads=[tmp, gsv, mods], writes=[tmp])
                        for bi in range(n // 128):
                            for k in range(8):
                                S.op("pe", lambda h, k=k, bi=bi: h.matmul(PD[1][:, 0:NEXP], tmp[:, k, bi * 128:(bi + 1) * 128], wrt[:, k, :], start=(k == 0), stop=(k == 7)),
                                     reads=[tmp, wrt], writes=[PR[2]], signal=(k == 7))
                            S.op("dve", lambda h: h.tensor_tensor(lg[:], PD[1][:, 0:NEXP], rowsb2[:, 0:NEXP], ALU.add), reads=[PR[2], rowsb2], writes=[lg])
                            S.op("dve", lambda h: h.tensor_reduce(m1[:], lg[:], AX.X, ALU.max), reads=[lg], writes=[m1])
                            S.op("dve", lambda h: h.tensor_scalar(k1[:], lg[:], m1[:, 0:1], None, ALU.is_equal), reads=[lg, m1], writes=[k1])
                            S.op("dve", lambda h: h.scalar_tensor_tensor(lg2[:], k1[:], -1e30, lg[:], ALU.mult, ALU.add), reads=[k1, lg], writes=[lg2])
                            S.op("dve", lambda h: h.tensor_reduce(m2[:], lg2[:], AX.X, ALU.max), reads=[lg2], writes=[m2])
                            S.op("dve", lambda h: h.tensor_scalar(k2[:], lg2[:], m2[:, 0:1], None, ALU.is_equal), reads=[lg2, m2], writes=[k2])
                            S.op("dve", lambda h: h.tensor_tensor(w1s[:], m1[:], m2[:], ALU.subtract), reads=[m1, m2], writes=[w1s])
                            S.op("act", lambda h: h.activation(w1s[:], w1s[:], AF.Sigmoid), reads=[w1s], writes=[w1s])
                            S.op("dve", lambda h: h.tensor_scalar(w2s[:], w1s[:], -1.0, 1.0, ALU.mult, ALU.add), reads=[w1s], writes=[w2s])
                            S.op("dve", lambda h: h.tensor_scalar(k1[:], k1[:], w1s[:, 0:1], None, ALU.mult), reads=[k1, w1s], writes=[k1])
                            S.op("dve", lambda h: h.scalar_tensor_tensor(cmb[:], k2[:], w2s[:, 0:1], k1[:], ALU.mult, ALU.add), reads=[k2, w2s, k1], writes=[cmb])
                            S.op("dve", lambda h: h.tensor_tensor(dg[:], cust(consts.t, 0, 128, 0, [(0, NEXP), (1, 128)]),
                                                                  cust(cmb.t, 0, 128, 0, [(1, NEXP), (0, 128)]), ALU.mult), reads=[consts, cmb], writes=[dg])
                            for hf in range(2):
                                S.op("pe", lambda h, hf=hf: h.matmul(PD[2 + hf][:, 0:512], onesf, dg[:, hf * 4:(hf + 1) * 4, :].rearrange("p a t -> p (a t)"), start=True, stop=True),
                                     reads=[consts, dg], writes=[PR[4 + 2 * hf]])
                                S.op("dve", lambda h, hf=hf: h.tensor_copy(cbt[:, hf * 4:(hf + 1) * 4, :], PD[2 + hf][:, 0:512].rearrange("p (a t) -> p a t", a=4)),
                                     reads=[PR[4 + 2 * hf]], writes=[cbt])
                            gq = t0 + bi * 128
                            S.dma("sp", comb_d[:, gq:gq + 128].rearrange("(o e) t -> o e t", o=1), cbt[0:1, :, :], reads=[cbt], writes=[comb_d])
                S.barrier()
            chk(5)
            with contextlib.ExitStack() as es:
                NG = 2
                FC = 11
                w1b = [sb(es, "w1b%d" % i, [128, 8, FC * 128], BF16) for i in range(2)]
                w3b = [sb(es, "w3b%d" % i, [128, 8, FC * 128], BF16) for i in range(2)]
                w2b = [sb(es, "w2b%d" % i, [128, FC, D], BF16) for i in range(2)]
                hTl = [sb(es, "hTl%d" % i, [128, 8, 512], BF16) for i in range(2)]
                gT = [sb(es, "gT%d" % i, [128, FC, 512], BF16) for i in range(2)]
                cbl = [sb(es, "cbl%d" % i, [128, 512]) for i in range(2)]
                sl = [sb(es, "sl%d" % i, [128, 512], BF16) for i in range(2)]
                yst = sb(es, "yst", [128, 8, 512])
                npass = NEXP * NG if moe else NG

                def wload(p_):
                    e_, g_ = (p_ // NG, p_ % NG) if moe else (None, p_)
                    f0 = g_ * FC * 128
                    wi = p_ % 2
                    if moe:
                        s1, s3, s2 = ew1_in[li, e_], ew3_in[li, e_], ew2_in[li, e_]
                    else:
                        s1, s3, s2 = fw1_in[li], fw3_in[li], fw2_in[li]
                    for k in range(8):
                        S.dma("pool", w1b[wi][:, k, :], s1[k * 128:(k + 1) * 128, f0:f0 + FC * 128], writes=[w1b[wi]])
                        S.dma("pool", w3b[wi][:, k, :], s3[k * 128:(k + 1) * 128, f0:f0 + FC * 128], writes=[w3b[wi]])
                    for fc in range(FC):
                        S.dma("pool", w2b[wi][:, fc, :], s2[f0 + fc * 128:f0 + (fc + 1) * 128, :], writes=[w2b[wi]])

                wload(0)
                for p_ in range(npass):
                    e_, g_ = (p_ // NG, p_ % NG) if moe else (None, p_)
                    wi = p_ % 2
                    if p_ + 1 < npass:
                        wload(p_ + 1)
                    for ti, (t0, n) in enumerate(tiles_c):
                        it = p_ * len(tiles_c) + ti
                        hl = hTl[it % 2]
                        S.dma("sp", hl[:, :, 0:n], h2_d[:, :, t0:t0 + n], reads=[h2_d], writes=[hl])
                        cb = cbl[it % 2]
                        if moe:
                            S.dma("sp", cb[:, 0:n], comb_d[e_:e_ + 1, t0:t0 + n].partition_broadcast(128), reads=[comb_d], writes=[cb])
                        g = gT[it % 2]
                        for fc in range(FC):
                            b1, b3 = (fc % 2) * 2, (fc % 2) * 2 + 1
                            for k in range(8):
                                S.op("pe", lambda h, k=k, fc=fc, b1=b1: h.matmul(pb(b1)[:, 0:n], w1b[wi][:, k, fc * 128:(fc + 1) * 128], hl[:, k, 0:n], start=(k == 0), stop=(k == 7)),
                                     reads=[w1b[wi], hl], writes=[PR[b1]], signal=(k == 7))
                            for k in range(8):
                                S.op("pe", lambda h, k=k, fc=fc, b3=b3: h.matmul(pb(b3)[:, 0:n], w3b[wi][:, k, fc * 128:(fc + 1) * 128], hl[:, k, 0:n], start=(k == 0), stop=(k == 7)),
                                     reads=[w3b[wi], hl], writes=[PR[b3]], signal=(k == 7))
                            s_ = sl[fc % 2]
                            S.op("act", lambda h, b1=b1, s_=s_: h.activation(s_[:, 0:n], pb(b1)[:, 0:n], AF.Silu), reads=[PR[b1]], writes=[s_])
                            if moe:
                                S.op("pool", lambda h, s_=s_: h.tensor_tensor(s_[:, 0:n], s_[:, 0:n], cb[:, 0:n], ALU.mult), reads=[s_, cb], writes=[s_])
                            S.op("dve", lambda h, fc=fc, b3=b3, s_=s_: h.tensor_tensor(g[:, fc, 0:n], pb(b3)[:, 0:n], s_[:, 0:n], ALU.mult),
                                 reads=[PR[b3], s_], writes=[g])
                        first = (p_ == 0)
                        for c in range(8):
                            bk = 4 + (c % 4)
                            for fc in range(FC):
                                S.op("pe", lambda h, c=c, fc=fc, bk=bk: h.matmul(pb(bk)[:, 0:n], w2b[wi][:, fc, c * 128:(c + 1) * 128], g[:, fc, 0:n], start=(fc == 0), stop=(fc == FC - 1)),
                                     reads=[w2b[wi], g], writes=[PR[bk]], signal=(fc == FC - 1))
                            if c % 2 == 0:
                                S.op("dve", lambda h, c=c, bk=bk: h.tensor_copy(yst[:, c, 0:n], pb(bk)[:, 0:n]), reads=[PR[bk]], writes=[yst])
                            else:
                                S.op("act", lambda h, c=c, bk=bk: h.activation(yst[:, c, 0:n], pb(bk)[:, 0:n], AF.Copy), reads=[PR[bk]], writes=[yst])
                        if first:
                            S.dma("pool", yacc_d[:, :, t0:t0 + n], yst[:, :, 0:n], reads=[yst], writes=[yacc_d])
                        else:
                            S.dma("pool", yacc_d[:, :, t0:t0 + n], yst[:, :, 0:n], reads=[yst], writes=[yacc_d], accum_op=ALU.add)
                S.barrier()
            chk(6)

        with contextlib.ExitStack() as es:
            Xs = [sb(es, "Xf%d" % i, [128, 8, 512]) for i in range(2)]
            Yfs = [sb(es, "Yf%d" % i, [128, 8, 512]) for i in range(2)]
            sq = sb(es, "sqf", [128, 8, 512], BF16)
            rstd = sb(es, "rstdf", [128, 512])
            tmp = sb(es, "tmpf", [128, 8, 512])
            yo = [sb(es, "yo%d" % i, [128, D]) for i in range(2)]
            oi = 0
            for ti, (t0, n) in enumerate(cfg.tiles):
                if t0 < CTX:
                    continue
                X = Xs[ti % 2]
                Yf = Yfs[ti % 2]
                S.dma("sp", X[:, :, 0:n], xT_d[:, :, t0:t0 + n], reads=[xT_d], writes=[X])
                S.dma("sp", Yf[:, :, 0:n], yacc_d[:, :, t0:t0 + n], reads=[yacc_d], writes=[Yf])
                for c in range(8):
                    S.op("dve", lambda h, c=c: h.scalar_tensor_tensor(X[:, c, 0:n], Yf[:, c, 0:n], mods[:, L - 1, 40 + c, 0:1], X[:, c, 0:n], ALU.mult, ALU.add),
                         reads=[Yf, mods, X], writes=[X])
                S.op("act", lambda h: h.activation(sq[:, :, 0:n], X[:, :, 0:n], AF.Square), reads=[X], writes=[sq])
                for k in range(8):
                    S.op("pe", lambda h, k=k: h.matmul(pb(0)[:, 0:n], onesb, sq[:, k, 0:n], start=(k == 0), stop=(k == 7)), reads=[cbf, sq], writes=[PR[0]], signal=(k == 7))
                S.op("act", lambda h: h.activation(rstd[:, 0:n], pb(0)[:, 0:n], AF.Sqrt, scale=1.0 / D, bias=epsb[:, 0:1]), reads=[PR[0], epsb], writes=[rstd])
                S.op("dve", lambda h: h.reciprocal(rstd[:, 0:n], rstd[:, 0:n]), reads=[rstd], writes=[rstd])
                for k in range(8):
                    S.op("dve", lambda h, k=k: h.scalar_tensor_tensor(tmp[:, k, 0:n], X[:, k, 0:n], vecs[:, 16 + k:17 + k], rstd[:, 0:n], ALU.mult, ALU.mult),
                         reads=[X, vecs, rstd], writes=[tmp])
                for bi in range(n // 128):
                    y_ = yo[oi % 2]
                    oi += 1
                    for c in range(8):
                        S.op("pe", lambda h, c=c, bi=bi: h.transpose(PD[1 + c // 4][:, (c % 4) * 128:(c % 4) * 128 + 128], tmp[:, c, bi * 128:(bi + 1) * 128], ident),
                             reads=[tmp, consts], writes=[PR[2 + (c // 4) * 2]], signal=(c % 4 == 3))
                    S.op("dve", lambda h, y_=y_: h.tensor_copy(y_[:, 0:512], PD[1][:, 0:512]), reads=[PR[2]], writes=[y_])
                    S.op("act", lambda h, y_=y_: h.activation(y_[:, 512:1024], PD[2][:, 0:512], AF.Copy), reads=[PR[4]], writes=[y_])
                    r0 = t0 - CTX + bi * 128
                    S.dma("sp", out_d[r0:r0 + 128, :], y_[:], reads=[y_])
      except StopBuild:
        pass
      S.barrier()
      S.run()
    except AssertionError:
        if cfg.stop == 99:
            raise
    S.close()
    return nc


def _fm(v):
    v = np.asarray(v, np.float32)
    return np.ascontiguousarray(v.reshape(-1, 128).T)


def _host_consts(S_):
    NT = CTX + S_
    quarter = 16
    inv = (10000.0 ** (-np.arange(quarter, dtype=np.float32) / quarter)).astype(np.float32)
    t = np.arange(S_)
    row = (t // 64).astype(np.float32)
    colp = (t % 64).astype(np.float32)
    ang_r = row[:, None] * inv[None, :]
    ang_c = colp[:, None] * inv[None, :]
    cos = np.ones((64, NT), np.float32)
    sin = np.zeros((64, NT), np.float32)
    for d in range(64):
        f = d % 16
        ang = ang_r[:, f] if d < 32 else ang_c[:, f]
        sgn = -1.0 if (d % 32) < 16 else 1.0
        cos[d, CTX:] = np.cos(ang)
        sin[d, CTX:] = sgn * np.sin(ang)
    cos2 = np.concatenate([cos, cos], 0)
    sin2 = np.concatenate([sin, sin], 0)
    tabs = np.stack([cos2 * 0.125, sin2 * 0.125, cos2, sin2]).astype(np.float32)
    consts = np.zeros((128, 768), np.float32)
    a = np.arange(128)
    consts[:, 0:128] = np.eye(128)
    consts[:, 128:256] = (a[:, None] <= a[None, :])
    consts[:, 256:384] = (a[:, None] >= a[None, :])
    consts[:, 384:512] = np.where(a[None, :] >= a[:, None], 0.0, NEG)
    consts[:, 512:640] = np.where(a[None, :] <= a[:, None], 0.0, NEG)
    consts[:, 640:768] = 1.0
    return tabs, consts


def _perm_cols():
    def partner(d):
        return d + 16 if (d % 32) < 16 else d - 16
    qa = np.arange(512)
    qa_p = np.array([(j // 64) * 64 + partner(j % 64) for j in range(512)])
    ka = 512 + np.arange(128)
    ka_p = 512 + np.array([(j // 64) * 64 + partner(j % 64) for j in range(128)])
    kd0 = np.concatenate([ka[0:64], ka[0:64]])
    kd1 = np.concatenate([ka[64:128], ka[64:128]])
    kd0p = np.concatenate([ka_p[0:64], ka_p[0:64]])
    kd1p = np.concatenate([ka_p[64:128], ka_p[64:128]])
    rest = np.concatenate([np.arange(768, 1280), np.arange(640, 768), np.arange(1280, 2320)])
    return np.concatenate([qa, qa_p, kd0, kd1, kd0p, kd1p, rest])


_CACHE = {}


def prepare_inputs(cfg, inp, ncores):
    L = cfg.depth
    tabs, consts = _host_consts(cfg.S)
    cols = _perm_cols()
    w_in = np.ascontiguousarray(np.asarray(inp["w_in"], np.float32)[:, :, cols])
    rows = np.zeros((L, RW), np.float32)
    rows[:, 0:16] = inp["b_gates"]
    rows[:, 16:24] = inp["attn_sink"]
    rows[:, 24:536] = inp["g_att"]
    rows[:, 536:1048] = inp["g_ml"]
    for l in range(L):
        if l % 2 == 1:
            rows[l, 1048:1056] = inp["b_router"][l // 2]
    nm1 = max(cfg.nm, 1)
    if cfg.nm > 0:
        wr = np.ascontiguousarray(np.asarray(inp["w_router"], np.float32).reshape(cfg.nm, 8, 128, NEXP).transpose(0, 2, 1, 3))
        ew1, ew3, ew2 = inp["exp_w1"], inp["exp_w3"], inp["exp_w2"]
    else:
        wr = np.zeros((1, 128, 8, NEXP), np.float32)
        ew1 = np.zeros((1, NEXP, D, FFN), np.float32)
        ew3 = ew1
        ew2 = np.zeros((1, NEXP, FFN, D), np.float32)
    maps = []
    for b in range(ncores):
        vecs = np.zeros((128, cfg.NV), np.float32)
        vecs[:, 0:8] = _fm(inp["c"][b])
        vecs[:, 8:16] = _fm(inp["c_ctx"])
        vecs[:, 16:24] = _fm(inp["final_g"])
        for l in range(L):
            vb = 24 + 88 * l
            vecs[:, vb:vb + 8] = _fm(inp["norm1_g"][l])
            vecs[:, vb + 8:vb + 16] = _fm(inp["norm2_g"][l])
            vecs[:, vb + 16:vb + 64] = _fm(inp["b_mod"][l])
            cw = np.asarray(inp["conv_w"][l], np.float32)
            for j in range(5):
                vecs[:, vb + 64 + j * 4:vb + 68 + j * 4] = _fm(cw[j])
            vecs[:, vb + 84:vb + 88] = _fm(inp["conv_b"][l])
        maps.append({
            "x": np.ascontiguousarray(inp["x"][b], dtype=np.float32), "ctx": np.ascontiguousarray(inp["ctx"][b], dtype=np.float32),
            "vecs": vecs, "rows": rows, "w_mod": np.asarray(inp["w_mod"], np.float32), "w_in": w_in,
            "w_out": np.asarray(inp["w_out"], np.float32),
            "ffn_w1": np.asarray(inp["ffn_w1"], np.float32), "ffn_w3": np.asarray(inp["ffn_w3"], np.float32),
            "ffn_w2": np.asarray(inp["ffn_w2"], np.float32), "w_router": wr,
            "exp_w1": np.asarray(ew1, np.float32), "exp_w3": np.asarray(ew3, np.float32), "exp_w2": np.asarray(ew2, np.float32),
            "tabs": tabs, "consts": consts,
        })
    return maps


def run(cfg, inp, ncores, trace=False):
    key = (cfg.S, cfg.depth)
    if key not in _CACHE:
        _CACHE[key] = build(cfg)
    nc = _CACHE[key]
    maps = prepare_inputs(cfg, inp, ncores)
    res = run_bass_kernel_spmd(nc, maps, core_ids=list(range(ncores)), trace=trace)
    out = np.stack([res.results[b]["out"] for b in range(ncores)], 0)
    return out, res


def kernel(**inputs):
    cfg = Cfg(S=4096, depth=4)
    out, _ = run(cfg, inputs, 8)
    return out.astype(np.float32)
```

```python
import contextlib
import numpy as np
import concourse.bass as bass
import concourse.mybir as mybir
from concourse.bass_utils import run_bass_kernel_spmd

F32 = mybir.dt.float32
BF16 = mybir.dt.bfloat16
AF = mybir.ActivationFunctionType
ALU = mybir.AluOpType
AX = mybir.AxisListType

D = 1024
CTX = 256
FFN = 2816
NEXP = 8
WIN = 3216
RW = 1056
EPS = 1e-6
NEG = -30000.0
LN8 = float(np.log(0.125))


class Res:
    __slots__ = ("name", "w", "r")

    def __init__(self, name):
        self.name = name
        self.w = None
        self.r = {}


class TT:
    def __init__(self, name, t):
        self.t = t
        self.res = Res(name)

    def __getitem__(self, k):
        return self.t[k]


def _res(x):
    return x.res if isinstance(x, TT) else x


def _freeze(fn):
    import types
    if fn.__closure__ is None:
        return fn
    cells = []
    for c in fn.__closure__:
        try:
            cells.append(types.CellType(c.cell_contents))
        except ValueError:
            cells.append(c)
    return types.FunctionType(fn.__code__, fn.__globals__, fn.__name__, fn.__defaults__, tuple(cells))


class Sched:
    ENGS = ("pe", "act", "dve", "pool", "sp")

    def __init__(self, nc, n_dma_sems=14):
        self.nc = nc
        self.sems = {}
        self.prog = {e: [] for e in self.ENGS}
        self.cnt = {e: 0 for e in self.ENGS}
        self.seen = {e: {} for e in self.ENGS}
        self.pending = {e: False for e in self.ENGS}
        self.nd = n_dma_sems
        self.dma_i = {e: 0 for e in self.ENGS}
        self.dma_val = {}
        self._ctx = []
        for e in self.ENGS:
            self._mksem("E_" + e)
        for q in ("sp", "pool"):
            for i in range(n_dma_sems):
                k = "D_%s_%d" % (q, i)
                self._mksem(k)
                self.dma_val[k] = 0

    def _mksem(self, key):
        cm = self.nc.semaphore(key)
        self.sems[key] = cm.__enter__()
        self._ctx.append(cm)

    def close(self):
        for cm in reversed(self._ctx):
            cm.__exit__(None, None, None)

    def _deps(self, eng, reads, writes):
        need = {}

        def add(tok, kind):
            if tok is None:
                return
            key, val, ename = tok
            if ename == eng and not key.startswith("D_"):
                if eng == "pe" or kind == "war":
                    return
            if need.get(key, 0) < val:
                need[key] = val

        for r in reads:
            add(_res(r).w, "raw")
        for w in writes:
            w = _res(w)
            add(w.w, "waw")
            for key, (val, ename) in w.r.items():
                add((key, val, ename), "war")
        out = []
        for key, val in need.items():
            if self.seen[eng].get(key, 0) >= val:
                continue
            self.seen[eng][key] = val
            out.append((key, val))
        return out

    def _record(self, tok, reads, writes):
        key, val, ename = tok
        for r in reads:
            _res(r).r[key] = (val, ename)
        for w in writes:
            w = _res(w)
            w.w = tok
            w.r = {}

    def op(self, eng, fn, reads=(), writes=(), signal=True):
        fn = _freeze(fn)
        if eng != "pe":
            signal = True
        waits = self._deps(eng, reads, writes)
        key = "E_" + eng
        val = self.cnt[eng] + 1
        sems = self.sems
        if signal:
            self.cnt[eng] = val
        self.pending[eng] = not signal

        def emit(h):
            for k, v in waits:
                h.wait_ge(sems[k], v)
            ins = fn(h)
            if signal:
                ins.then_inc(sems[key], 1)

        self.prog[eng].append(emit)
        self._record((key, val, eng), reads, writes)

    def dma(self, q, out, in_, reads=(), writes=(), **kw):
        i = self.dma_i[q] % self.nd
        self.dma_i[q] += 1
        dkey = "D_%s_%d" % (q, i)
        prev = self.dma_val[dkey]
        waits = self._deps(q, reads, writes)
        if prev > 0 and self.seen[q].get(dkey, 0) < prev:
            self.seen[q][dkey] = prev
            waits.append((dkey, prev))
        val = prev + 16
        self.dma_val[dkey] = val
        sems = self.sems

        def emit(h):
            for k, v in waits:
                h.wait_ge(sems[k], v)
            h.dma_start(out=out, in_=in_, **kw).then_inc(sems[dkey], 16)

        self.prog[q].append(emit)
        self._record((dkey, val, q), reads, writes)

    def barrier(self):
        tgt = {("E_" + e): self.cnt[e] for e in self.ENGS if self.cnt[e] > 0}
        for k, v in self.dma_val.items():
            if v > 0:
                tgt[k] = v
        sems = self.sems
        for e in self.ENGS:
            assert not self.pending[e]
            ws = []
            for k, v in tgt.items():
                if k == "E_" + e and e == "pe":
                    continue
                if self.seen[e].get(k, 0) < v:
                    self.seen[e][k] = v
                    ws.append((k, v))

            def emit(h, ws=ws):
                for k, v in ws:
                    h.wait_ge(sems[k], v)

            self.prog[e].append(emit)

    def run(self):
        nc = self.nc
        for e in self.ENGS:
            assert not self.pending[e], e
        prog = self.prog
        with nc.Block() as block:
            @block.tensor
            def _(h):
                for f in prog["pe"]:
                    f(h)

            @block.scalar
            def _(h):
                for f in prog["act"]:
                    f(h)

            @block.vector
            def _(h):
                for f in prog["dve"]:
                    f(h)

            @block.gpsimd
            def _(h):
                for f in prog["pool"]:
                    f(h)

            @block.sync
            def _(h):
                for f in prog["sp"]:
                    f(h)


def cust(t, pstart, np_, off, dims):
    base = t[:]
    pstep = base.ap[0][0]
    return bass.AP(t, base.offset + pstart * pstep + off, [[pstep, np_]] + [[s, c] for s, c in dims])


class StopBuild(Exception):
    pass


class Cfg:
    def __init__(self, S=4096, depth=4, dbg=False, stop=99):
        self.stop = stop
        self.S = S
        self.depth = depth
        self.NT = CTX + S
        self.NB = self.NT // 128
        self.tiles = [(0, 256)] + [(CTX + 512 * i, 512) for i in range(S // 512)]
        self.nd = (depth + 1) // 2
        self.nm = depth // 2
        self.NV = 24 + 88 * depth
        self.dbg = dbg


def build(cfg):
    S_, L, NT, NB = cfg.S, cfg.depth, cfg.NT, cfg.NB
    nc = bass.Bass("TRN2", target_bir_lowering=False)

    def din(name, shape, dt=F32):
        return nc.dram_tensor(name, list(shape), dt, kind="ExternalInput").ap()

    x_in = din("x", [S_, D])
    ctx_in = din("ctx", [CTX, D])
    vecs_in = din("vecs", [128, cfg.NV])
    rows_in = din("rows", [L, RW])
    wmod_in = din("w_mod", [L, D, 6 * D])
    win_in = din("w_in", [L, D, WIN])
    wout_in = din("w_out", [L, D, D])
    fw1_in = din("ffn_w1", [cfg.nd, D, FFN])
    fw3_in = din("ffn_w3", [cfg.nd, D, FFN])
    fw2_in = din("ffn_w2", [cfg.nd, FFN, D])
    nm1 = max(cfg.nm, 1)
    wr_in = din("w_router", [nm1, 128, 8, NEXP])
    ew1_in = din("exp_w1", [nm1, NEXP, D, FFN])
    ew3_in = din("exp_w3", [nm1, NEXP, D, FFN])
    ew2_in = din("exp_w2", [nm1, NEXP, FFN, D])
    tabs_in = din("tabs", [4, 128, NT])
    consts_in = din("consts", [128, 768])
    out_d = nc.dram_tensor("out", [S_, D], F32, kind="ExternalOutput").ap()

    def dscr(name, shape, dt=F32):
        return TT(name, nc.dram_tensor(name, list(shape), dt).ap())

    xT_d = dscr("xT_d", [128, 8, NT])
    pcd_d = dscr("pcd_d", [128, 4, NT + 16])
    qT_d = dscr("qT_d", [128, 4, NT], BF16)
    qm_d = dscr("qm_d", [128, 2, NT], BF16)
    km_d = dscr("km_d", [128, 2, NT], BF16)
    kmtok_d = dscr("kmtok_d", [NB, 128, 256], BF16)
    vm_d = dscr("vm_d", [NB, 128, 516], BF16)
    og_d = dscr("og_d", [NB, 128, 512], BF16)
    h2_d = dscr("h2_d", [128, 8, NT], BF16)
    comb_d = dscr("comb_d", [NEXP, NT])
    yacc_d = dscr("yacc_d", [128, 8, NT])

    S = Sched(nc)
    es_all = contextlib.ExitStack()

    _uid = [0]

    def sb(es, name, shape, dt=F32):
        _uid[0] += 1
        nm = "s%d_%s" % (_uid[0], name)
        return TT(nm, es.enter_context(nc.sbuf_tensor(nm, list(shape), dt)))

    def pc_col(g):
        return g + 4 if g < CTX else g + 8

    try:
     with es_all:
      es0 = es_all
      try:
        def chk(k):
            if cfg.stop == k:
                raise StopBuild()
        PD = [es0.enter_context(nc.psum_tensor("pd%d" % i, [128, 1024], F32)) for i in range(4)]
        PR = [Res("pb%d" % i) for i in range(8)]

        def pb(i):
            return PD[i // 2][:, (i % 2) * 512:(i % 2) * 512 + 512]

        vecs = sb(es0, "vecs", [128, cfg.NV])
        consts = sb(es0, "consts", [128, 768])
        cbf = sb(es0, "cbf", [128, 512], BF16)
        mods = sb(es0, "mods", [128, L, 48, 2])
        gsv = sb(es0, "gsv", [128, L, 2, 8, 2])
        condT = sb(es0, "condT", [128, 8, 2])
        S.dma("sp", vecs[:], vecs_in[:, :], writes=[vecs])
        S.dma("sp", consts[:], consts_in[:, :], writes=[consts])
        ident = consts[:, 0:128]
        trif = consts[:, 128:256]
        trib = consts[:, 256:384]
        onesf = consts[:, 640:768]
        S.op("dve", lambda h: h.tensor_copy(cbf[:, 0:128], consts[:, 0:128]), reads=[consts], writes=[cbf])
        S.op("dve", lambda h: h.tensor_copy(cbf[:, 128:384], consts[:, 384:640]), reads=[consts], writes=[cbf])
        S.op("dve", lambda h: h.tensor_copy(cbf[:, 384:512], consts[:, 640:768]), reads=[consts], writes=[cbf])
        identb = cbf[:, 0:128]
        maskAb = cbf[:, 128:256]
        maskBb = cbf[:, 256:384]
        onesb = cbf[:, 384:512]

        with contextlib.ExitStack() as es:
            wm = [sb(es, "wm%d" % i, [128, 8, 1024]) for i in range(2)]
            xin = [sb(es, "xin%d" % i, [128, D]) for i in range(2)]
            xo = [sb(es, "xo%d" % i, [128, 8, 128]) for i in range(2)]
            zt = sb(es, "zt", [128, 4, 16])
            S.op("act", lambda h: h.activation(condT[:, :, 0], vecs[:, 0:8], AF.Silu), reads=[vecs], writes=[condT])
            S.op("act", lambda h: h.activation(condT[:, :, 1], vecs[:, 8:16], AF.Silu), reads=[vecs], writes=[condT])
            pi = 0
            for l in range(L):
                for m in range(6):
                    w = wm[pi % 2]
                    pi += 1
                    S.dma("sp", w[:], wmod_in[l, :, m * 1024:(m + 1) * 1024].rearrange("(k p) n -> p k n", p=128), writes=[w])
                    bk = 6
                    for c in range(8):
                        for k in range(8):
                            S.op("pe", lambda h, w=w, c=c, k=k: h.matmul(PD[3][:, c * 2:c * 2 + 2], w[:, k, c * 128:(c + 1) * 128], condT[:, k, :],
                                                                        start=(k == 0), stop=(k == 7)),
                                 reads=[w, condT], writes=[PR[bk]], signal=(k == 7 and c == 7))
                    bcol = 24 + 88 * l + 16 + m * 8
                    S.op("dve", lambda h, l=l, m=m, bcol=bcol: h.tensor_tensor(
                        mods[:, l, m * 8:(m + 1) * 8, :], PD[3][:, 0:16].rearrange("p (c t) -> p c t", t=2),
                        cust(vecs.t, 0, 128, bcol, [(1, 8), (0, 2)]), ALU.add), reads=[PR[bk], vecs], writes=[mods])
                for j, (gcol, mi) in enumerate(((24 + 88 * l, 1), (24 + 88 * l + 8, 4))):
                    S.op("dve", lambda h, l=l, j=j, gcol=gcol, mi=mi: h.scalar_tensor_tensor(
                        gsv[:, l, j, :, :], mods[:, l, mi * 8:(mi + 1) * 8, :], 1.0,
                        cust(vecs.t, 0, 128, gcol, [(1, 8), (0, 2)]), ALU.add, ALU.mult), reads=[mods, vecs], writes=[gsv])
            S.op("dve", lambda h: h.memset(zt[:], 0.0), writes=[zt])
            for c0 in (0, 260, 264 + S_):
                w_ = 4 if c0 != 264 + S_ else 8
                S.dma("sp", pcd_d[:, :, c0:c0 + w_], zt[:, :, 0:w_], reads=[zt], writes=[pcd_d])
            for b in range(NB):
                g0 = b * 128
                xi = xin[b % 2]
                src = ctx_in[g0:g0 + 128, :] if g0 < CTX else x_in[g0 - CTX:g0 - CTX + 128, :]
                S.dma("sp", xi[:], src, writes=[xi])
                for c in range(8):
                    S.op("pe", lambda h, xi=xi, c=c: h.transpose(PD[c // 4][:, (c % 4) * 128:(c % 4) * 128 + 128], xi[:, c * 128:(c + 1) * 128], ident),
                         reads=[xi, consts], writes=[PR[(c // 4) * 2]], signal=(c % 4 == 3))
                o = xo[b % 2]
                S.op("dve", lambda h, o=o: h.tensor_copy(o[:, 0:4, :], PD[0][:, 0:512].rearrange("p (c n) -> p c n", c=4)), reads=[PR[0]], writes=[o])
                S.op("act", lambda h, o=o: h.activation(o[:, 4:8, :], PD[1][:, 0:512].rearrange("p (c n) -> p c n", c=4), AF.Copy), reads=[PR[2]], writes=[o])
                S.dma("sp", xT_d[:, :, g0:g0 + 128], o[:], reads=[o], writes=[xT_d])
        S.barrier()
        chk(0)

        def adaln(X, n, sq, rstd, tmp, hT, l, which, ci, bank):
            S.op("act", lambda h: h.activation(sq[:, :, 0:n], X[:, :, 0:n], AF.Square), reads=[X], writes=[sq])
            chk(30)
            for k in range(8):
                S.op("pe", lambda h, k=k: h.matmul(pb(bank)[:, 0:n], onesb, sq[:, k, 0:n], start=(k == 0), stop=(k == 7)),
                     reads=[cbf, sq], writes=[PR[bank]], signal=(k == 7))
            chk(31)
            S.op("act", lambda h: h.activation(rstd[:, 0:n], pb(bank)[:, 0:n], AF.Sqrt, scale=1.0 / D, bias=epsb[:, 0:1]), reads=[PR[bank], epsb], writes=[rstd])
            chk(32)
            S.op("dve", lambda h: h.reciprocal(rstd[:, 0:n], rstd[:, 0:n]), reads=[rstd], writes=[rstd])
            S.op("dve", lambda h: h.tensor_tensor(tmp[:, :, 0:n], X[:, :, 0:n], cust(rstd.t, 0, 128, 0, [(0, 8), (1, n)]), ALU.mult),
                 reads=[X, rstd], writes=[tmp])
            chk(33)
            shift_i = 0 if which == 0 else 3
            for k in range(8):
                S.op("dve", lambda h, k=k: h.tensor_scalar(hT[:, k, 0:n], tmp[:, k, 0:n], gsv[:, l, which, k, ci:ci + 1],
                                                           mods[:, l, shift_i * 8 + k, ci:ci + 1], ALU.mult, ALU.add),
                     reads=[tmp, mods, gsv], writes=[hT], signal=(k == 7))

        epsb = sb(es0, "epsb", [128, 1])
        S.op("dve", lambda h: h.memset(epsb[:], EPS), writes=[epsb])
        ln8b = sb(es0, "ln8b", [128, 1])
        S.op("dve", lambda h: h.memset(ln8b[:], LN8), writes=[ln8b])

        for l in range(L):
            vb = 24 + 88 * l
            with contextlib.ExitStack() as esAB:
                KT2 = sb(esAB, "KT2", [128, 2, NT], BF16)
                Vt = sb(esAB, "Vt", [128, NB, 128], BF16)
                Graw = sb(esAB, "Graw", [128, NB, 16])
                rowsb = sb(esAB, "rowsb", [128, RW])
                S.dma("sp", rowsb[:], rows_in[l:l + 1, :].partition_broadcast(128), writes=[rowsb])
                with contextlib.ExitStack() as es:
                    winb = sb(es, "winb", [128, 8, WIN], BF16)
                    for k in range(8):
                        S.dma("pool", winb[:, k, :], win_in[l, k * 128:(k + 1) * 128, :], writes=[winb])
                    Xs = [sb(es, "Xa%d" % i, [128, 8, 512]) for i in range(1)]
                    Ya = sb(es, "Ya", [128, 8, 512])
                    sq = sb(es, "sqa", [128, 8, 512], BF16)
                    rstd = sb(es, "rstda", [128, 512])
                    tmp = sb(es, "tmpa", [128, 8, 512])
                    hT = sb(es, "hTa", [128, 8, 512], BF16)
                    tb = [sb(es, "tb%d" % i, [128, 4, 512]) for i in range(1)]
                    r1 = sb(es, "r1", [128, 512])
                    r2 = sb(es, "r2", [128, 512])
                    qst = [sb(es, "qst%d" % i, [128, 4, 512], BF16) for i in range(2)]
                    pcs = [sb(es, "pcs%d" % i, [128, 4, 512]) for i in range(1)]
                    vms = [sb(es, "vms%d" % i, [128, 4, 129], BF16) for i in range(2)]
                    ogs = [sb(es, "ogs%d" % i, [128, 512], BF16) for i in range(2)]
                    sg = sb(es, "sg", [128, 512])
                    for i in range(2):
                        S.op("dve", lambda h, i=i: h.memset(vms[i][:, :, 128:129], 1.0), writes=[vms[i]])
                    chk(20)
                    for ti, (t0, n) in enumerate(cfg.tiles):
                        ci = 1 if t0 < CTX else 0
                        X = Xs[0]
                        S.dma("sp", X[:, :, 0:n], xT_d[:, :, t0:t0 + n], reads=[xT_d], writes=[X])
                        if l > 0:
                            S.dma("sp", Ya[:, :, 0:n], yacc_d[:, :, t0:t0 + n], reads=[yacc_d], writes=[Ya])
                            for c in range(8):
                                S.op("dve", lambda h, c=c: h.scalar_tensor_tensor(X[:, c, 0:n], Ya[:, c, 0:n], mods[:, l - 1, 40 + c, ci:ci + 1], X[:, c, 0:n], ALU.mult, ALU.add),
                                     reads=[Ya, mods, X], writes=[X])
                            S.dma("sp", xT_d[:, :, t0:t0 + n], X[:, :, 0:n], reads=[X], writes=[xT_d])
                        tbt = tb[0]
                        S.dma("sp", tbt[:, :, 0:n], tabs_in[:, :, t0:t0 + n].rearrange("f p n -> p f n"), writes=[tbt])
                        chk(21)
                        adaln(X, n, sq, rstd, tmp, hT, l, 0, ci, 0)
                        chk(10)
                        qs_ = qst[ti % 2]
                        pc = pcs[0]

                        def proj(chunk, bank):
                            for k in range(8):
                                S.op("pe", lambda h, k=k: h.matmul(pb(bank)[:, 0:n], winb[:, k, chunk * 128:(chunk + 1) * 128], hT[:, k, 0:n],
                                                                   start=(k == 0), stop=(k == 7)),
                                     reads=[winb, hT], writes=[PR[bank]], signal=(k == 7))

                        for qi in range(6):
                            ca = qi if qi < 4 else 8 + (qi - 4)
                            cp = 4 + qi if qi < 4 else 10 + (qi - 4)
                            ba, bp = 2 + (qi % 2) * 2, 3 + (qi % 2) * 2
                            proj(ca, ba)
                            proj(cp, bp)
                            tcs = (0, 1) if qi < 4 else (2, 3)
                            S.op("dve", lambda h, ba=ba, tcs=tcs: h.tensor_tensor(r1[:, 0:n], pb(ba)[:, 0:n], tbt[:, tcs[0], 0:n], ALU.mult),
                                 reads=[PR[ba], tbt], writes=[r1])
                            S.op("dve", lambda h, bp=bp, tcs=tcs: h.tensor_tensor(r2[:, 0:n], pb(bp)[:, 0:n], tbt[:, tcs[1], 0:n], ALU.mult),
                                 reads=[PR[bp], tbt], writes=[r2])
                            if qi < 4:
                                S.op("pool", lambda h, qi=qi: h.tensor_tensor(qs_[:, qi, 0:n], r1[:, 0:n], r2[:, 0:n], ALU.add),
                                     reads=[r1, r2], writes=[qs_])
                            else:
                                S.op("pool", lambda h, qi=qi: h.tensor_tensor(KT2[:, qi - 4, t0:t0 + n], r1[:, 0:n], r2[:, 0:n], ALU.add),
                                     reads=[r1, r2], writes=[KT2])
                        S.dma("sp", qT_d[:, :, t0:t0 + n], qs_[:, :, 0:n], reads=[qs_], writes=[qT_d])
                        chk(11)
                        for mi in range(4):
                            bk = 2 + (mi % 2)
                            proj(12 + mi, bk)
                            if mi % 2 == 0:
                                S.op("dve", lambda h, mi=mi, bk=bk: h.tensor_copy(pc[:, mi, 0:n], pb(bk)[:, 0:n]), reads=[PR[bk]], writes=[pc])
                            else:
                                S.op("act", lambda h, mi=mi, bk=bk: h.activation(pc[:, mi, 0:n], pb(bk)[:, 0:n], AF.Copy), reads=[PR[bk]], writes=[pc])
                        c0 = pc_col(t0)
                        S.dma("sp", pcd_d[:, :, c0:c0 + n], pc[:, :, 0:n], reads=[pc], writes=[pcd_d])
                        chk(12)
                        for bi in range(n // 128):
                            blk = (t0 + bi * 128) // 128
                            for gi, (c_lo, c_n, bk) in enumerate(((2048, 128, 6), (2176, 512, 4), (2688, 512, 5), (3200, 16, 7))):
                                for k in range(8):
                                    S.op("pe", lambda h, k=k, c_lo=c_lo, c_n=c_n, bk=bk: h.matmul(
                                        pb(bk)[:, 0:c_n], hT[:, k, bi * 128:(bi + 1) * 128], winb[:, k, c_lo:c_lo + c_n], start=(k == 0), stop=(k == 7)),
                                         reads=[hT, winb], writes=[PR[bk]], signal=(k == 7))
                            S.op("act", lambda h, blk=blk: h.activation(Vt[:, blk, :], pb(6)[:, 0:128], AF.Copy), reads=[PR[6]], writes=[Vt])
                            vmt = vms[blk % 2]
                            S.op("dve", lambda h, vmt=vmt: h.tensor_copy(vmt[:, :, 0:128], pb(4)[:, 0:512].rearrange("p (a e) -> p a e", a=4)),
                                 reads=[PR[4]], writes=[vmt])
                            S.dma("sp", vm_d[blk, :, :], vmt[:].rearrange("p a e -> p (a e)"), reads=[vmt], writes=[vm_d])
                            S.op("act", lambda h: h.activation(sg[:], pb(5)[:, 0:512], AF.Sigmoid), reads=[PR[5]], writes=[sg])
                            ogt = ogs[blk % 2]
                            S.op("dve", lambda h, ogt=ogt: h.tensor_tensor(ogt[:], sg[:], rowsb[:, 536:1048], ALU.mult), reads=[sg, rowsb], writes=[ogt])
                            S.dma("sp", og_d[blk, :, :], ogt[:], reads=[ogt], writes=[og_d])
                            S.op("dve", lambda h, blk=blk: h.tensor_tensor(Graw[:, blk, :], pb(7)[:, 0:16], rowsb[:, 0:16], ALU.add),
                                 reads=[PR[7], rowsb], writes=[Graw])
                S.barrier()
                chk(1)
                with contextlib.ExitStack() as es:
                    win_ = [sb(es, "cw%d" % i, [128, 4, 516]) for i in range(2)]
                    acc = sb(es, "cacc", [128, 4, 512])
                    qk = [sb(es, "cqk%d" % i, [128, 4, 512], BF16) for i in range(2)]
                    ktk = [sb(es, "ktk%d" % i, [128, 4, 256], BF16) for i in range(2)]
                    for ti, (t0, n) in enumerate(cfg.tiles):
                        wv = win_[ti % 2]
                        c0 = pc_col(t0)
                        S.dma("sp", wv[:, :, 0:n + 4], pcd_d[:, :, c0 - 2:c0 + n + 2], reads=[pcd_d], writes=[wv])
                        q_ = qk[ti % 2]
                        for c in range(4):
                            S.op("dve", lambda h, c=c: h.tensor_scalar(acc[:, c, 0:n], wv[:, c, 0:n], vecs[:, vb + 64 + c:vb + 65 + c],
                                                                       vecs[:, vb + 84 + c:vb + 85 + c], ALU.mult, ALU.add),
                                 reads=[wv, vecs], writes=[acc])
                            for j in range(1, 5):
                                S.op("dve", lambda h, c=c, j=j: h.scalar_tensor_tensor(acc[:, c, 0:n], wv[:, c, j:j + n],
                                                                                       vecs[:, vb + 64 + j * 4 + c:vb + 65 + j * 4 + c],
                                                                                       acc[:, c, 0:n], ALU.mult, ALU.add),
                                     reads=[wv, vecs, acc], writes=[acc])
                        S.op("act", lambda h: h.activation(q_[:, :, 0:n], acc[:, :, 0:n], AF.Silu), reads=[acc], writes=[q_])
                        S.dma("sp", qm_d[:, :, t0:t0 + n], q_[:, 0:2, 0:n], reads=[q_], writes=[qm_d])
                        S.dma("sp", km_d[:, :, t0:t0 + n], q_[:, 2:4, 0:n], reads=[q_], writes=[km_d])
                        kt_ = ktk[ti % 2]
                        nb_ = n // 128
                        PTb = PD[0][:, 0:512].bitcast(BF16)
                        for bi in range(nb_):
                            for c in range(2):
                                S.op("pe", lambda h, bi=bi, c=c: h.transpose(PTb[:, (bi * 2 + c) * 128:(bi * 2 + c + 1) * 128],
                                                                             q_[:, 2 + c, bi * 128:(bi + 1) * 128], identb),
                                     reads=[q_, cbf], writes=[PR[0]], signal=(bi == nb_ - 1 and c == 1))
                        S.op("dve", lambda h, nb_=nb_: h.tensor_copy(kt_[:, 0:nb_, :], PTb[:, 0:nb_ * 256].rearrange("p (b c) -> p b c", c=256)),
                             reads=[PR[0]], writes=[kt_])
                        blk0 = t0 // 128
                        S.dma("sp", kmtok_d[blk0:blk0 + nb_, :, :].rearrange("b p c -> p b c"), kt_[:, 0:nb_, :], reads=[kt_], writes=[kmtok_d])
                S.barrier()
                chk(2)

                with contextlib.ExitStack() as es:
                    woutb = sb(es, "woutb", [128, 8, D], BF16)
                    for k in range(8):
                        S.dma("pool", woutb[:, k, :], wout_in[l, k * 128:(k + 1) * 128, :], writes=[woutb])
                    G = sb(es, "G", [128, NB, 16])
                    LF = sb(es, "LF", [128, NB, 2, 4])
                    LI = sb(es, "LI", [128, NB, 2, 4])
                    Bc = sb(es, "Bc", [128, NB, 8])
                    Aa = sb(es, "Aa", [128, NB, 8])
                    WK = sb(es, "WK", [128, NB, 8])
                    DEC = sb(es, "DEC", [128, NB, 2, 2])
                    BT2 = sb(es, "BT2", [128, NB, 8])
                    S.op("act", lambda h: h.activation(G[:], Graw[:], AF.Tanh, scale=1.0 / 15.0), reads=[Graw], writes=[G])
                    S.op("dve", lambda h: h.tensor_scalar(G[:], G[:], 15.0, None, ALU.mult), reads=[G], writes=[G])
                    Gv = G[:].rearrange("p b (k a) -> p b k a", k=4)
                    for d_ in range(2):
                        S.op("dve", lambda h, d_=d_: h.tensor_copy(LI[:, :, d_, :], Gv[:, :, 2 * d_, :]), reads=[G], writes=[LI])
                        S.op("act", lambda h, d_=d_: h.activation(LF[:, :, d_, :], Gv[:, :, 2 * d_ + 1, :], AF.Exp, scale=-1.0), reads=[G], writes=[LF])
                    S.op("act", lambda h: h.activation(LF[:], LF[:], AF.Ln, bias=1.0), reads=[LF], writes=[LF])
                    S.op("dve", lambda h: h.tensor_scalar(LF[:], LF[:], -1.0, None, ALU.mult), reads=[LF], writes=[LF])
                    for b in range(NB):
                        S.op("pe", lambda h, b=b: h.matmul(PD[0][:, 0:4], trif, LF[:, b, 0, :], start=True, stop=True), reads=[consts, LF], writes=[PR[0]], signal=False)
                        S.op("pe", lambda h, b=b: h.matmul(PD[0][:, 4:8], trib, LF[:, b, 1, :], start=True, stop=True), reads=[consts, LF], writes=[PR[0]], signal=False)
                        S.op("pe", lambda h, b=b: h.matmul(PD[0][:, 8:16], onesf, LF[:, b, :, :].rearrange("p a b -> p (a b)"), start=True, stop=True),
                             reads=[consts, LF], writes=[PR[0]])
                        S.op("dve", lambda h, b=b: h.tensor_copy(Bc[:, b, :], PD[0][:, 0:8]), reads=[PR[0]], writes=[Bc])
                        S.op("dve", lambda h, b=b: h.tensor_copy(BT2[:, b, :], PD[0][:, 8:16]), reads=[PR[0]], writes=[BT2])
                    LIf = LI[:].rearrange("p b a c -> p b (a c)")
                    S.op("dve", lambda h: h.tensor_tensor(Aa[:], LIf, Bc[:], ALU.subtract), reads=[LI, Bc], writes=[Aa])
                    S.op("dve", lambda h: h.tensor_tensor(WK[:], Aa[:], BT2[:], ALU.add), reads=[Aa, BT2], writes=[WK])
                    S.op("act", lambda h: h.activation(WK[:], WK[:], AF.Exp), reads=[WK], writes=[WK])
                    S.op("dve", lambda h: h.tensor_scalar(Aa[:], Aa[:], LN8, None, ALU.add), reads=[Aa], writes=[Aa])
                    for half in range(2):
                        S.op("act", lambda h, half=half: h.activation(
                            DEC[half * 64:(half + 1) * 64, :, :, :],
                            cust(BT2.t, half * 64, 64, half, [(8, NB), (4, 2), (2, 2)]), AF.Exp), reads=[BT2], writes=[DEC])

                    CS = sb(es, "CS", [128, NB, 2, 2, 129], BF16)
                    Cst = sb(es, "Cst", [128, 2, 2, 129])
                    S.op("dve", lambda h: h.memset(Cst[:], 0.0), writes=[Cst])
                    order_f = list(range(NB))
                    order_b = [1, 0] + list(range(NB - 1, 1, -1))
                    kws = [sb(es, "kw%d" % i, [128, 2, 4, 64], BF16) for i in range(2)]
                    ktl = [sb(es, "ktl%d" % i, [128, 2, 256], BF16) for i in range(2)]
                    vml = [sb(es, "vml%d" % i, [128, 2, 516], BF16) for i in range(2)]
                    for i in range(NB):
                        fb, bb = order_f[i], order_b[i]
                        kt_, vm_, kw = ktl[i % 2], vml[i % 2], kws[i % 2]
                        S.dma("sp", kt_[:, 0, :], kmtok_d[fb, :, :], reads=[kmtok_d], writes=[kt_])
                        S.dma("sp", kt_[:, 1, :], kmtok_d[bb, :, :], reads=[kmtok_d], writes=[kt_])
                        S.dma("sp", vm_[:, 0, :], vm_d[fb, :, :], reads=[vm_d], writes=[vm_])
                        S.dma("sp", vm_[:, 1, :], vm_d[bb, :, :], reads=[vm_d], writes=[vm_])
                        S.op("act", lambda h, fb=fb: h.activation(CS[:, fb, 0, :, :], Cst[:, 0, :, :], AF.Copy), reads=[Cst], writes=[CS])
                        S.op("act", lambda h, bb=bb: h.activation(CS[:, bb, 1, :, :], Cst[:, 1, :, :], AF.Copy), reads=[Cst], writes=[CS])
                        for d_, blk in ((0, fb), (1, bb)):
                            S.op("dve", lambda h, d_=d_, blk=blk: h.tensor_tensor(
                                kw[:, d_, :, :], kt_[:, d_, :].rearrange("p (a e) -> p a e", a=4),
                                cust(WK.t, 0, 128, blk * 8 + d_ * 4, [(1, 4), (0, 64)]), ALU.mult), reads=[kt_, WK], writes=[kw])
                        for d_ in range(2):
                            for hd in range(4):
                                bank = 2 + d_
                                S.op("pe", lambda h, d_=d_, hd=hd, bank=bank: h.matmul(
                                    pb(bank)[(hd % 2) * 64:(hd % 2) * 64 + 64, (hd // 2) * 129:(hd // 2) * 129 + 129],
                                    kw[:, d_, hd, :], vm_[:, d_, hd * 129:(hd + 1) * 129], start=True, stop=True),
                                     reads=[kw, vm_], writes=[PR[bank]], signal=(hd == 3))
                        for d_, blk in ((0, fb), (1, bb)):
                            S.op("dve", lambda h, d_=d_, blk=blk: h.tensor_tensor(
                                Cst[:, d_, :, :], Cst[:, d_, :, :], cust(DEC.t, 0, 128, blk * 4 + d_ * 2, [(1, 2), (0, 129)]), ALU.mult),
                                 reads=[Cst, DEC], writes=[Cst])
                            S.op("dve", lambda h, d_=d_: h.tensor_tensor(
                                Cst[:, d_, :, :], Cst[:, d_, :, :], pb(2 + d_)[:, 0:258].rearrange("p (a e) -> p a e", a=2), ALU.add),
                                 reads=[Cst, PR[2 + d_]], writes=[Cst])

                    S.barrier()
                    chk(3)
                    qtl = [sb(es, "qtl%d" % i, [128, 4, 128], BF16) for i in range(2)]
                    qml = [sb(es, "qml%d" % i, [128, 2, 128], BF16) for i in range(2)]
                    kml = [sb(es, "kml%d" % i, [128, 2, 128], BF16) for i in range(2)]
                    vm1 = [sb(es, "vm1%d" % i, [128, 516], BF16) for i in range(2)]
                    ogl = [sb(es, "ogl%d" % i, [128, 512], BF16) for i in range(2)]
                    xbl = [sb(es, "xbl%d" % i, [128, 8, 128]) for i in range(3)]
                    Pm = [sb(es, "Pm%d" % i, [128, 640], BF16) for i in range(2)]
                    PTs = [sb(es, "PTs%d" % i, [128, 5, 128], BF16) for i in range(2)]
                    rmax = sb(es, "rmax", [128, 8])
                    negm = sb(es, "negm", [128, 8])
                    rs = sb(es, "rs", [128, 8])
                    es_ = sb(es, "es_", [128, 8])
                    att = sb(es, "att", [128, 8, 64])
                    ss1 = sb(es, "ss1", [128, 1])
                    junk = sb(es, "junk", [128, 512])
                    cats = [sb(es, "cat%d" % i, [128, D], BF16) for i in range(2)]
                    catT = sb(es, "catT", [128, 8, 128], BF16)
                    Rm = sb(es, "Rm", [128, 8, 128])
                    Em = sb(es, "Em", [128, 8, 128], BF16)
                    Emm = sb(es, "Emm", [128, 8, 128], BF16)
                    PTm = sb(es, "PTm", [128, 8, 128], BF16)
                    EB = sb(es, "EB", [128, 8, 128])
                    QS = sb(es, "QS", [128, 2, 2, 128], BF16)
                    den = sb(es, "den", [128, 2, 4])
                    hsum = sb(es, "hsum", [128, 4, 128])
                    h2 = sb(es, "h2", [128, 4, 128])
                    ss4 = sb(es, "ss4", [128, 4])
                    t512 = sb(es, "t512", [128, 512])

                    def loads(b):
                        g0 = b * 128
                        S.dma("sp", qtl[b % 2][:], qT_d[:, :, g0:g0 + 128], reads=[qT_d], writes=[qtl[b % 2]])
                        S.dma("sp", qml[b % 2][:], qm_d[:, :, g0:g0 + 128], reads=[qm_d], writes=[qml[b % 2]])
                        S.dma("sp", kml[b % 2][:], km_d[:, :, g0:g0 + 128], reads=[km_d], writes=[kml[b % 2]])
                        S.dma("sp", vm1[b % 2][:], vm_d[b, :, :], reads=[vm_d], writes=[vm1[b % 2]])
                        S.dma("sp", ogl[b % 2][:], og_d[b, :, :], reads=[og_d], writes=[ogl[b % 2]])
                        S.dma("sp", xbl[b % 3][:], xT_d[:, :, g0:g0 + 128], reads=[xT_d], writes=[xbl[b % 3]])

                    mg = [None]

                    def merge_gen(b):
                        g0 = b * 128
                        cat_ = cats[b % 2]
                        xb_ = xbl[b % 3]
                        ci = 1 if g0 < CTX else 0
                        PTb = PD[0][:, :].bitcast(BF16)
                        for c in range(8):
                            S.op("pe", lambda h, c=c: h.transpose(PTb[:, c * 128:(c + 1) * 128], cat_[:, c * 128:(c + 1) * 128], identb),
                                 reads=[cat_, cbf], writes=[PR[0]], signal=(c == 7))
                        yield
                        S.op("dve", lambda h: h.tensor_copy(catT[:], PTb[:, 0:1024].rearrange("p (c t) -> p c t", c=8)), reads=[PR[0]], writes=[catT])
                        yield
                        for c in range(8):
                            for k in range(8):
                                S.op("pe", lambda h, c=c, k=k: h.matmul(PD[1][:, c * 128:(c + 1) * 128], woutb[:, k, c * 128:(c + 1) * 128], catT[:, k, :],
                                                                        start=(k == 0), stop=(k == 7), skip_group_check=True),
                                     reads=[woutb, catT], writes=[PR[2 + c // 4]], signal=(k == 7 and c % 4 == 3))
                            if c % 2 == 1:
                                yield
                        for c in range(8):
                            S.op("dve", lambda h, c=c: h.scalar_tensor_tensor(xb_[:, c, :], PD[1][:, c * 128:(c + 1) * 128], mods[:, l, 16 + c, ci:ci + 1],
                                                                              xb_[:, c, :], ALU.mult, ALU.add),
                                 reads=[PR[2 + c // 4], mods, xb_], writes=[xb_])
                            if c % 4 == 3:
                                yield
                        S.dma("sp", xT_d[:, :, g0:g0 + 128], xb_[:], reads=[xb_], writes=[xT_d])

                    def tick():
                        if mg[0] is not None:
                            try:
                                next(mg[0])
                            except StopIteration:
                                mg[0] = None

                    def flush():
                        while mg[0] is not None:
                            tick()

                    loads(0)
                    for b in range(NB):
                        if b + 1 < NB:
                            loads(b + 1)
                        g0 = b * 128
                        if l == L - 1 and g0 < CTX:
                            continue
                        qt, qm_, km_, vmb, og, xb = qtl[b % 2], qml[b % 2], kml[b % 2], vm1[b % 2], ogl[b % 2], xbl[b % 3]
                        cat = cats[b % 2]
                        if g0 < CTX:
                            segs = [(0, CTX, None)]
                        else:
                            j = (g0 - CTX) // 128
                            segs = []
                            if j > 0:
                                segs.append((g0 - 128, 128, "A"))
                            segs.append((g0, 128, None))
                            if j < S_ // 128 - 1:
                                segs.append((g0 + 128, 128, "B"))
                            segs.append((0, CTX, None))
                        nk = sum(s[1] for s in segs)
                        def _hd_vars(hd):
                            kv = hd // 4
                            ph = (hd % 2) * 64
                            pd_i = 2 + (hd % 2)
                            return kv, ph, pd_i, pd_i * 2, pd_i * 2 + 1, PD[pd_i]

                        def att_scores(hd):
                            kv = hd // 4
                            ph = (hd % 2) * 64
                            pd_i = 2 + (hd % 2)
                            bA, bB = pd_i * 2, pd_i * 2 + 1
                            SC = PD[pd_i]
                            col = 0
                            mms = []
                            for (k0, kn, mk) in segs:
                                pieces = []
                                c_, k_, r_ = col, k0, kn
                                while r_ > 0:
                                    w_ = min(r_, 512 - (c_ % 512)) if c_ < 512 else r_
                                    pieces.append((c_, k_, w_))
                                    c_ += w_
                                    k_ += w_
                                    r_ -= w_
                                for (c_, k_, w_) in pieces:
                                    mms.append(("s", c_, k_, w_))
                                if mk is not None:
                                    mms.append(("m", col, mk, 128))
                                col += kn
                            started = set()
                            for mi_, (kind, c_, k_, w_) in enumerate(mms):
                                bank = bA if c_ < 512 else bB
                                st = bank not in started
                                started.add(bank)
                                last = mi_ == len(mms) - 1
                                if kind == "s":
                                    S.op("pe", lambda h, c_=c_, k_=k_, w_=w_, st=st: h.matmul(
                                        SC[:, c_:c_ + w_], qt[ph:ph + 64, hd // 2, :], KT2[ph:ph + 64, kv, k_:k_ + w_], start=st, stop=False, skip_group_check=True),
                                         reads=[qt, KT2], writes=[PR[bank]], signal=last)
                                else:
                                    mt = maskAb if k_ == "A" else maskBb
                                    S.op("pe", lambda h, c_=c_, mt=mt, st=st: h.matmul(SC[:, c_:c_ + 128], identb, mt, start=st, stop=False, skip_group_check=True),
                                         reads=[cbf], writes=[PR[bank]], signal=last)

                        def att_rest(hd):
                            kv, ph, pd_i, bA, bB, SC = _hd_vars(hd)
                            rd = [PR[bA]] + ([PR[bB]] if nk > 512 else [])
                            S.op("dve", lambda h, hd=hd: h.tensor_reduce(rmax[:, hd:hd + 1], SC[:, 0:nk], AX.X, ALU.max), reads=rd, writes=[rmax])
                            S.op("dve", lambda h, hd=hd: h.tensor_scalar(negm[:, hd:hd + 1], rmax[:, hd:hd + 1], rowsb[:, 16 + hd:17 + hd], -1.0, ALU.max, ALU.mult),
                                 reads=[rmax, rowsb], writes=[negm])
                            P_ = Pm[hd % 2]
                            S.op("act", lambda h, hd=hd, P_=P_: h.activation(P_[:, 0:nk], SC[:, 0:nk], AF.Exp, bias=negm[:, hd:hd + 1], accum_out=rs[:, hd:hd + 1]),
                                 reads=rd + [negm], writes=[P_, rs])
                            nkb = nk // 128
                            PTb = PD[0][:, (hd % 2) * 512:(hd % 2) * 512 + 512].bitcast(BF16)
                            for kb in range(nkb):
                                S.op("pe", lambda h, kb=kb, P_=P_: h.transpose(PTb[:, kb * 128:(kb + 1) * 128], P_[:, kb * 128:(kb + 1) * 128], identb),
                                     reads=[P_, cbf], writes=[PR[hd % 2]], signal=(kb == nkb - 1))
                            PT_ = PTs[hd % 2]
                            S.op("act" if hd % 2 else "dve",
                                 (lambda h, PT_=PT_, PTb=PTb, nkb=nkb: h.activation(PT_[:, 0:nkb, :], PTb[:, 0:nkb * 128].rearrange("p (a q) -> p a q", q=128), AF.Copy)) if hd % 2 else
                                 (lambda h, PT_=PT_, PTb=PTb, nkb=nkb: h.tensor_copy(PT_[:, 0:nkb, :], PTb[:, 0:nkb * 128].rearrange("p (a q) -> p a q", q=128))),
                                 reads=[PR[hd % 2]], writes=[PT_])
                            kb = 0
                            for (k0, kn, mk) in segs:
                                for s_ in range(kn // 128):
                                    kblk = (k0 + s_ * 128) // 128
                                    S.op("pe", lambda h, kb=kb, kblk=kblk, hd=hd, kv=kv: h.matmul(
                                        PD[1][:, hd * 64:(hd + 1) * 64], PT_[:, kb, :], Vt[:, kblk, kv * 64:(kv + 1) * 64], start=(kb == 0), stop=(kb == nkb - 1),
                                        skip_group_check=True),
                                         reads=[PT_, Vt], writes=[PR[2]], signal=(kb == nkb - 1))
                                    kb += 1

                        att_scores(0)
                        for hd in range(8):
                            if hd + 1 < 8:
                                att_scores(hd + 1)
                            att_rest(hd)
                        S.op("dve", lambda h: h.tensor_tensor(es_[:], rowsb[:, 16:24], negm[:], ALU.add), reads=[rowsb, negm], writes=[es_])
                        S.op("act", lambda h: h.activation(es_[:], es_[:], AF.Exp), reads=[es_], writes=[es_])
                        S.op("dve", lambda h: h.tensor_tensor(rs[:], rs[:], es_[:], ALU.add), reads=[rs, es_], writes=[rs])
                        S.op("dve", lambda h: h.reciprocal(rs[:], rs[:]), reads=[rs], writes=[rs])
                        S.op("dve", lambda h: h.tensor_tensor(att[:], PD[1][:, 0:512].rearrange("p (a e) -> p a e", a=8),
                                                              cust(rs.t, 0, 128, 0, [(1, 8), (0, 64)]), ALU.mult), reads=[PR[2], rs], writes=[att])
                        attf = att[:].rearrange("p a e -> p (a e)")
                        S.op("act", lambda h: h.activation(junk[:], attf, AF.Square, accum_out=ss1[:]), reads=[att], writes=[junk, ss1])
                        S.op("act", lambda h: h.activation(ss1[:], ss1[:], AF.Sqrt, scale=1.0 / 512, bias=epsb[:, 0:1]), reads=[ss1, epsb], writes=[ss1])
                        S.op("dve", lambda h: h.reciprocal(ss1[:], ss1[:]), reads=[ss1], writes=[ss1])
                        S.op("dve", lambda h: h.scalar_tensor_tensor(cat[:, 0:512], attf, ss1[:, 0:1], rowsb[:, 24:536], ALU.mult, ALU.mult),
                             reads=[att, ss1, rowsb], writes=[cat])

                        chk(40)
                        for hd in (0, 2, 1, 3):
                            ph = (hd % 2) * 64
                            S.op("pe", lambda h, hd=hd, ph=ph: h.matmul(pb(4 + hd % 2)[:, (hd // 2) * 128:(hd // 2 + 1) * 128], km_[ph:ph + 64, hd // 2, :], qm_[ph:ph + 64, hd // 2, :],
                                                                        start=(hd // 2 == 0), stop=(hd // 2 == 1), skip_group_check=True),
                                 reads=[km_, qm_], writes=[PR[4 + hd % 2]], signal=(hd // 2 == 1))
                        tick()
                        S.op("dve", lambda h, b=b: h.tensor_tensor(
                            Rm[:].rearrange("p (d a) t -> p d a t", d=2), cust(consts.t, 0, 128, 128, [(128, 2), (0, 4), (1, 128)]),
                            cust(LF.t, 0, 128, b * 8, [(4, 2), (1, 4), (0, 128)]), ALU.mult), reads=[consts, LF], writes=[Rm])
                        for d_ in range(2):
                            S.op("pe", lambda h, d_=d_: h.matmul(PD[3][:, d_ * 512:(d_ + 1) * 512], onesf, Rm[:, d_ * 4:(d_ + 1) * 4, :].rearrange("p a t -> p (a t)"),
                                                                 start=True, stop=True), reads=[consts, Rm], writes=[PR[6 + d_]])
                        chk(50)
                        tick()
                        for dh in range(8):
                            S.op("act", lambda h, dh=dh, b=b: h.activation(Em[:, dh, :], PD[3][:, dh * 128:(dh + 1) * 128], AF.Exp, bias=Aa[:, b, dh:dh + 1]),
                                 reads=[PR[6 + dh // 4], Aa], writes=[Em], signal=(dh == 7))
                        S.op("act", lambda h: h.activation(EB[:].rearrange("p a t -> p (a t)"), PD[3][:, :], AF.Exp, bias=ln8b[:, 0:1]),
                             reads=[PR[6], PR[7], ln8b], writes=[EB])
                        chk(51)
                        tick()
                        S.op("pool", lambda h: h.affine_select(Emm[:, 0:4, :], Em[:, 0:4, :], [[0, 4], [1, 128]], ALU.is_ge, 0.0, base=0, channel_multiplier=-1),
                             reads=[Em], writes=[Emm])
                        S.op("pool", lambda h: h.affine_select(Emm[:, 4:8, :], Em[:, 4:8, :], [[0, 4], [-1, 128]], ALU.is_ge, 0.0, base=0, channel_multiplier=1),
                             reads=[Em], writes=[Emm])
                        for d_ in range(2):
                            for par in range(2):
                                S.op("dve", lambda h, d_=d_, par=par: h.tensor_tensor(
                                    cust(PTm.t, 0, 128, (d_ * 4 + par) * 128, [(256, 2), (1, 128)]), pb(4 + par)[:, 0:256].rearrange("p (a t) -> p a t", a=2),
                                    cust(Emm.t, 0, 128, (d_ * 4 + par) * 128, [(256, 2), (1, 128)]), ALU.mult), reads=[PR[4 + par], Emm], writes=[PTm])
                        tick()
                        for half in range(2):
                            S.op("dve", lambda h, half=half: h.tensor_tensor(
                                QS[half * 64:(half + 1) * 64, :, :, :], cust(qm_.t, half * 64, 64, 0, [(0, 2), (128, 2), (1, 128)]),
                                cust(EB.t, half * 64, 64, half * 128, [(512, 2), (256, 2), (1, 128)]), ALU.mult), reads=[qm_, EB], writes=[QS])
                        chk(52)
                        tick()
                        for d_ in range(2):
                            for hd in range(4):
                                ph = (hd % 2) * 64
                                bank = 4 + 0
                                o = PD[2][:, 0:1024] if False else None
                                dst = PD[2 if d_ == 0 else 3]
                                col = (hd // 2) * 512 + (hd % 2) * 129
                                bk = (4 if d_ == 0 else 6) + hd // 2
                                S.op("pe", lambda h, d_=d_, hd=hd, dst=dst, col=col: h.matmul(
                                    dst[:, col:col + 129], PTm[:, d_ * 4 + hd, :], vmb[:, hd * 129:(hd + 1) * 129], start=True, stop=False, skip_group_check=True),
                                     reads=[PTm, vmb], writes=[PR[bk]], signal=False)
                                S.op("pe", lambda h, d_=d_, hd=hd, dst=dst, col=col, ph=ph, b=b: h.matmul(
                                    dst[:, col:col + 129], QS[ph:ph + 64, d_, hd // 2, :], CS[ph:ph + 64, b, d_, hd // 2, :], start=False, stop=True, skip_group_check=True),
                                     reads=[QS, CS], writes=[PR[bk]], signal=(hd % 2 == 1))
                        chk(53)
                        tick()
                        for d_ in range(2):
                            src = PD[2 if d_ == 0 else 3]
                            bks = [PR[4], PR[5]] if d_ == 0 else [PR[6], PR[7]]
                            S.op("act", lambda h, d_=d_, src=src: h.activation(
                                den[:, d_, :].rearrange("p (a c) -> p a c", a=2), cust(src, 0, 128, 128, [(512, 2), (129, 2)]), AF.Abs),
                                 reads=bks, writes=[den])
                        S.op("dve", lambda h: h.tensor_scalar(den[:], den[:], 1.0, None, ALU.max), reads=[den], writes=[den])
                        S.op("dve", lambda h: h.reciprocal(den[:], den[:]), reads=[den], writes=[den])
                        for d_ in range(2):
                            src = PD[2 if d_ == 0 else 3]
                            bks = [PR[4], PR[5]] if d_ == 0 else [PR[6], PR[7]]
                            dstt = hsum if d_ == 0 else h2
                            for hh in range(2):
                                S.op("dve", lambda h, d_=d_, hh=hh, src=src, dstt=dstt: h.tensor_tensor(
                                    dstt[:, hh * 2:hh * 2 + 2, :], cust(src, 0, 128, hh * 512, [(129, 2), (1, 128)]),
                                    cust(den.t, 0, 128, d_ * 4 + hh * 2, [(1, 2), (0, 128)]), ALU.mult), reads=[bks[hh], den], writes=[dstt])
                        S.op("pool", lambda h: h.tensor_tensor(hsum[:], hsum[:], h2[:], ALU.add), reads=[hsum, h2], writes=[hsum])
                        tick()
                        S.op("pool", lambda h: h.tensor_tensor(h2[:], hsum[:], hsum[:], ALU.mult), reads=[hsum], writes=[h2])
                        S.op("dve", lambda h: h.tensor_reduce(ss4[:], h2[:], AX.X, ALU.add), reads=[h2], writes=[ss4])
                        S.op("act", lambda h: h.activation(ss4[:], ss4[:], AF.Sqrt, scale=1.0 / 128, bias=epsb[:, 0:1]), reads=[ss4, epsb], writes=[ss4])
                        S.op("dve", lambda h: h.reciprocal(ss4[:], ss4[:]), reads=[ss4], writes=[ss4])
                        S.op("dve", lambda h: h.tensor_tensor(t512[:].rearrange("p (a e) -> p a e", a=4), hsum[:], cust(ss4.t, 0, 128, 0, [(1, 4), (0, 128)]), ALU.mult),
                             reads=[hsum, ss4], writes=[t512])
                        S.op("dve", lambda h: h.tensor_tensor(cat[:, 512:1024], t512[:], og[:], ALU.mult), reads=[t512, og], writes=[cat])
                        chk(41)
                        flush()
                        mg[0] = merge_gen(b)
                    flush()
                S.barrier()
            chk(4)

            moe = (l % 2 == 1)
            li = l // 2
            tiles_c = cfg.tiles[1:] if l == L - 1 else cfg.tiles
            with contextlib.ExitStack() as es:
                Xs = [sb(es, "Xc%d" % i, [128, 8, 512]) for i in range(2)]
                sqs = [sb(es, "sqc%d" % i, [128, 8, 512], BF16) for i in range(2)]
                rstds = [sb(es, "rstdc%d" % i, [128, 512]) for i in range(2)]
                tmps = [sb(es, "tmpc%d" % i, [128, 8, 512]) for i in range(2)]
                hTs = [sb(es, "hTc%d" % i, [128, 8, 512], BF16) for i in range(2)]
                if moe:
                    wrt = sb(es, "wrt", [128, 8, NEXP])
                    S.dma("sp", wrt[:], wr_in[li, :, :, :], writes=[wrt])
                    rowsb2 = sb(es, "rowsb2", [128, NEXP])
                    S.dma("sp", rowsb2[:], rows_in[l:l + 1, 1048:1056].partition_broadcast(128), writes=[rowsb2])
                    lg = sb(es, "lg", [128, NEXP])
                    m1 = sb(es, "m1", [128, 1])
                    m2 = sb(es, "m2", [128, 1])
                    k1 = sb(es, "k1", [128, NEXP])
                    k2 = sb(es, "k2", [128, NEXP])
                    lg2 = sb(es, "lg2", [128, NEXP])
                    w1s = sb(es, "w1s", [128, 1])
                    w2s = sb(es, "w2s", [128, 1])
                    cmb = sb(es, "cmb", [128, NEXP])
                    dg = sb(es, "dg", [128, NEXP, 128])
                    cbt = sb(es, "cbt", [128, NEXP, 128])
                for ti, (t0, n) in enumerate(tiles_c):
                    ci = 1 if t0 < CTX else 0
                    X = Xs[ti % 2]
                    sq, rstd, tmp, hT = sqs[ti % 2], rstds[ti % 2], tmps[ti % 2], hTs[ti % 2]
                    S.dma("sp", X[:, :, 0:n], xT_d[:, :, t0:t0 + n], reads=[xT_d], writes=[X])
                    adaln(X, n, sq, rstd, tmp, hT, l, 1, ci, ti % 2)
                    S.dma("sp", h2_d[:, :, t0:t0 + n], hT[:, :, 0:n], reads=[hT], writes=[h2_d])
                    if moe:
                        for k in range(8):
                            S.op("dve", lambda h, k=k: h.tensor_scalar(tmp[:, k, 0:n], tmp[:, k, 0:n], gsv[:, l, 1, k, ci:ci + 1], mods[:, l, 24 + k, ci:ci + 1], ALU.mult, ALU.add),
                                 reads=[tmp, gsv, mods], writes=[tmp])
                        for bi in range(n // 128):
                            for k in range(8):
                                S.op("pe", lambda h, k=k, bi=bi: h.matmul(PD[1][:, 0:NEXP], tmp[:, k, bi * 128:(bi + 1) * 128], wrt[:, k, :], start=(k == 0), stop=(k == 7)),
                                     reads=[tmp, wrt], writes=[PR[2]], signal=(k == 7))
                            S.op("dve", lambda h: h.tensor_tensor(lg[:], PD[1][:, 0:NEXP], rowsb2[:, 0:NEXP], ALU.add), reads=[PR[2], rowsb2], writes=[lg])
                            S.op("dve", lambda h: h.tensor_reduce(m1[:], lg[:], AX.X, ALU.max), reads=[lg], writes=[m1])
                            S.op("dve", lambda h: h.tensor_scalar(k1[:], lg[:], m1[:, 0:1], None, ALU.is_equal), reads=[lg, m1], writes=[k1])
                            S.op("dve", lambda h: h.scalar_tensor_tensor(lg2[:], k1[:], -1e30, lg[:], ALU.mult, ALU.add), reads=[k1, lg], writes=[lg2])
                            S.op("dve", lambda h: h.tensor_reduce(m2[:], lg2[:], AX.X, ALU.max), reads=[lg2], writes=[m2])
                            S.op("dve", lambda h: h.tensor_scalar(k2[:], lg2[:], m2[:, 0:1], None, ALU.is_equal), reads=[lg2, m2], writes=[k2])
                            S.op("dve", lambda h: h.tensor_tensor(w1s[:], m1[:], m2[:], ALU.subtract), reads=[m1, m2], writes=[w1s])
                            S.op("act", lambda h: h.activation(w1s[:], w1s[:], AF.Sigmoid), reads=[w1s], writes=[w1s])
                            S.op("dve", lambda h: h.tensor_scalar(w2s[:], w1s[:], -1.0, 1.0, ALU.mult, ALU.add), reads=[w1s], writes=[w2s])
                            S.op("dve", lambda h: h.tensor_scalar(k1[:], k1[:], w1s[:, 0:1], None, ALU.mult), reads=[k1, w1s], writes=[k1])
                            S.op("dve", lambda h: h.scalar_tensor_tensor(cmb[:], k2[:], w2s[:, 0:1], k1[:], ALU.mult, ALU.add), reads=[k2, w2s, k1], writes=[cmb])
                            S.op("dve", lambda h: h.tensor_tensor(dg[:], cust(consts.t, 0, 128, 0, [(0, NEXP), (1, 128)]),
                                                                  cust(cmb.t, 0, 128, 0, [(1, NEXP), (0, 128)]), ALU.mult), reads=[consts, cmb], writes=[dg])
                            for hf in range(2):
                                S.op("pe", lambda h, hf=hf: h.matmul(PD[2 + hf][:, 0:512], onesf, dg[:, hf * 4:(hf + 1) * 4, :].rearrange("p a t -> p (a t)"), start=True, stop=True),
                                     reads=[consts, dg], writes=[PR[4 + 2 * hf]])
                                S.op("dve", lambda h, hf=hf: h.tensor_copy(cbt[:, hf * 4:(hf + 1) * 4, :], PD[2 + hf][:, 0:512].rearrange("p (a t) -> p a t", a=4)),
                                     reads=[PR[4 + 2 * hf]], writes=[cbt])
                            gq = t0 + bi * 128
                            S.dma("sp", comb_d[:, gq:gq + 128].rearrange("(o e) t -> o e t", o=1), cbt[0:1, :, :], reads=[cbt], writes=[comb_d])
                S.barrier()
            chk(5)
            with contextlib.ExitStack() as es:
                NG = 2
                FC = 11
                w1b = [sb(es, "w1b%d" % i, [128, 8, FC * 128], BF16) for i in range(2)]
                w3b = [sb(es, "w3b%d" % i, [128, 8, FC * 128], BF16) for i in range(2)]
                w2b = [sb(es, "w2b%d" % i, [128, FC, D], BF16) for i in range(2)]
                hTl = [sb(es, "hTl%d" % i, [128, 8, 512], BF16) for i in range(2)]
                gT = [sb(es, "gT%d" % i, [128, FC, 512], BF16) for i in range(2)]
                cbl = [sb(es, "cbl%d" % i, [128, 512]) for i in range(2)]
                sl = [sb(es, "sl%d" % i, [128, 512], BF16) for i in range(2)]
                yst = sb(es, "yst", [128, 8, 512])
                npass = NEXP * NG if moe else NG

                def wload(p_):
                    e_, g_ = (p_ // NG, p_ % NG) if moe else (None, p_)
                    f0 = g_ * FC * 128
                    wi = p_ % 2
                    if moe:
                        s1, s3, s2 = ew1_in[li, e_], ew3_in[li, e_], ew2_in[li, e_]
                    else:
                        s1, s3, s2 = fw1_in[li], fw3_in[li], fw2_in[li]
                    for k in range(8):
                        S.dma("pool", w1b[wi][:, k, :], s1[k * 128:(k + 1) * 128, f0:f0 + FC * 128], writes=[w1b[wi]])
                        S.dma("pool", w3b[wi][:, k, :], s3[k * 128:(k + 1) * 128, f0:f0 + FC * 128], writes=[w3b[wi]])
                    for fc in range(FC):
                        S.dma("pool", w2b[wi][:, fc, :], s2[f0 + fc * 128:f0 + (fc + 1) * 128, :], writes=[w2b[wi]])

                wload(0)
                for p_ in range(npass):
                    e_, g_ = (p_ // NG, p_ % NG) if moe else (None, p_)
                    wi = p_ % 2
                    if p_ + 1 < npass:
                        wload(p_ + 1)
                    for ti, (t0, n) in enumerate(tiles_c):
                        it = p_ * len(tiles_c) + ti
                        hl = hTl[it % 2]
                        S.dma("sp", hl[:, :, 0:n], h2_d[:, :, t0:t0 + n], reads=[h2_d], writes=[hl])
                        cb = cbl[it % 2]
                        if moe:
                            S.dma("sp", cb[:, 0:n], comb_d[e_:e_ + 1, t0:t0 + n].partition_broadcast(128), reads=[comb_d], writes=[cb])
                        g = gT[it % 2]
                        for fc in range(FC):
                            b1, b3 = (fc % 2) * 2, (fc % 2) * 2 + 1
                            for k in range(8):
                                S.op("pe", lambda h, k=k, fc=fc, b1=b1: h.matmul(pb(b1)[:, 0:n], w1b[wi][:, k, fc * 128:(fc + 1) * 128], hl[:, k, 0:n], start=(k == 0), stop=(k == 7)),
                                     reads=[w1b[wi], hl], writes=[PR[b1]], signal=(k == 7))
                            for k in range(8):
                                S.op("pe", lambda h, k=k, fc=fc, b3=b3: h.matmul(pb(b3)[:, 0:n], w3b[wi][:, k, fc * 128:(fc + 1) * 128], hl[:, k, 0:n], start=(k == 0), stop=(k == 7)),
                                     reads=[w3b[wi], hl], writes=[PR[b3]], signal=(k == 7))
                            s_ = sl[fc % 2]
                            S.op("act", lambda h, b1=b1, s_=s_: h.activation(s_[:, 0:n], pb(b1)[:, 0:n], AF.Silu), reads=[PR[b1]], writes=[s_])
                            if moe:
                                S.op("pool", lambda h, s_=s_: h.tensor_tensor(s_[:, 0:n], s_[:, 0:n], cb[:, 0:n], ALU.mult), reads=[s_, cb], writes=[s_])
                            S.op("dve", lambda h, fc=fc, b3=b3, s_=s_: h.tensor_tensor(g[:, fc, 0:n], pb(b3)[:, 0:n], s_[:, 0:n], ALU.mult),
                                 reads=[PR[b3], s_], writes=[g])
                        first = (p_ == 0)
                        for c in range(8):
                            bk = 4 + (c % 4)
                            for fc in range(FC):
                                S.op("pe", lambda h, c=c, fc=fc, bk=bk: h.matmul(pb(bk)[:, 0:n], w2b[wi][:, fc, c * 128:(c + 1) * 128], g[:, fc, 0:n], start=(fc == 0), stop=(fc == FC - 1)),
                                     reads=[w2b[wi], g], writes=[PR[bk]], signal=(fc == FC - 1))
                            if c % 2 == 0:
                                S.op("dve", lambda h, c=c, bk=bk: h.tensor_copy(yst[:, c, 0:n], pb(bk)[:, 0:n]), reads=[PR[bk]], writes=[yst])
                            else:
                                S.op("act", lambda h, c=c, bk=bk: h.activation(yst[:, c, 0:n], pb(bk)[:, 0:n], AF.Copy), reads=[PR[bk]], writes=[yst])
                        if first:
                            S.dma("pool", yacc_d[:, :, t0:t0 + n], yst[:, :, 0:n], reads=[yst], writes=[yacc_d])
                        else:
                            S.dma("pool", yacc_d[:, :, t0:t0 + n], yst[:, :, 0:n], reads=[yst], writes=[yacc_d], accum_op=ALU.add)
                S.barrier()
            chk(6)

        with contextlib.ExitStack() as es:
            Xs = [sb(es, "Xf%d" % i, [128, 8, 512]) for i in range(2)]
            Yfs = [sb(es, "Yf%d" % i, [128, 8, 512]) for i in range(2)]
            sqs = [sb(es, "sqf%d" % i, [128, 8, 512], BF16) for i in range(2)]
            rstds = [sb(es, "rstdf%d" % i, [128, 512]) for i in range(2)]
            tmps = [sb(es, "tmpf%d" % i, [128, 8, 512]) for i in range(2)]
            yo = [sb(es, "yo%d" % i, [128, D]) for i in range(2)]
            oi = 0
            for ti, (t0, n) in enumerate(cfg.tiles):
                if t0 < CTX:
                    continue
                X = Xs[ti % 2]
                Yf = Yfs[ti % 2]
                sq, rstd, tmp = sqs[ti % 2], rstds[ti % 2], tmps[ti % 2]
                fb_ = ti % 2
                S.dma("sp", X[:, :, 0:n], xT_d[:, :, t0:t0 + n], reads=[xT_d], writes=[X])
                S.dma("sp", Yf[:, :, 0:n], yacc_d[:, :, t0:t0 + n], reads=[yacc_d], writes=[Yf])
                for c in range(8):
                    S.op("dve", lambda h, c=c: h.scalar_tensor_tensor(X[:, c, 0:n], Yf[:, c, 0:n], mods[:, L - 1, 40 + c, 0:1], X[:, c, 0:n], ALU.mult, ALU.add),
                         reads=[Yf, mods, X], writes=[X])
                S.op("act", lambda h: h.activation(sq[:, :, 0:n], X[:, :, 0:n], AF.Square), reads=[X], writes=[sq])
                for k in range(8):
                    S.op("pe", lambda h, k=k: h.matmul(pb(fb_)[:, 0:n], onesb, sq[:, k, 0:n], start=(k == 0), stop=(k == 7)), reads=[cbf, sq], writes=[PR[fb_]], signal=(k == 7))
                S.op("act", lambda h: h.activation(rstd[:, 0:n], pb(fb_)[:, 0:n], AF.Sqrt, scale=1.0 / D, bias=epsb[:, 0:1]), reads=[PR[fb_], epsb], writes=[rstd])
                S.op("dve", lambda h: h.reciprocal(rstd[:, 0:n], rstd[:, 0:n]), reads=[rstd], writes=[rstd])
                for k in range(8):
                    S.op("dve", lambda h, k=k: h.scalar_tensor_tensor(tmp[:, k, 0:n], X[:, k, 0:n], vecs[:, 16 + k:17 + k], rstd[:, 0:n], ALU.mult, ALU.mult),
                         reads=[X, vecs, rstd], writes=[tmp])
                for bi in range(n // 128):
                    y_ = yo[oi % 2]
                    oi += 1
                    for c in range(8):
                        S.op("pe", lambda h, c=c, bi=bi: h.transpose(PD[1 + c // 4][:, (c % 4) * 128:(c % 4) * 128 + 128], tmp[:, c, bi * 128:(bi + 1) * 128], ident),
                             reads=[tmp, consts], writes=[PR[2 + (c // 4) * 2]], signal=(c % 4 == 3))
                    S.op("dve", lambda h, y_=y_: h.tensor_copy(y_[:, 0:512], PD[1][:, 0:512]), reads=[PR[2]], writes=[y_])
                    S.op("act", lambda h, y_=y_: h.activation(y_[:, 512:1024], PD[2][:, 0:512], AF.Copy), reads=[PR[4]], writes=[y_])
                    r0 = t0 - CTX + bi * 128
                    S.dma("sp", out_d[r0:r0 + 128, :], y_[:], reads=[y_])
      except StopBuild:
        pass
      S.barrier()
      S.run()
    except AssertionError:
        if cfg.stop == 99:
            raise
    S.close()
    return nc


def _fm(v):
    v = np.asarray(v, np.float32)
    return np.ascontiguousarray(v.reshape(-1, 128).T)


def _host_consts(S_):
    NT = CTX + S_
    quarter = 16
    inv = (10000.0 ** (-np.arange(quarter, dtype=np.float32) / quarter)).astype(np.float32)
    t = np.arange(S_)
    row = (t // 64).astype(np.float32)
    colp = (t % 64).astype(np.float32)
    ang_r = row[:, None] * inv[None, :]
    ang_c = colp[:, None] * inv[None, :]
    cos = np.ones((64, NT), np.float32)
    sin = np.zeros((64, NT), np.float32)
    for d in range(64):
        f = d % 16
        ang = ang_r[:, f] if d < 32 else ang_c[:, f]
        sgn = -1.0 if (d % 32) < 16 else 1.0
        cos[d, CTX:] = np.cos(ang)
        sin[d, CTX:] = sgn * np.sin(ang)
    cos2 = np.concatenate([cos, cos], 0)
    sin2 = np.concatenate([sin, sin], 0)
    tabs = np.stack([cos2 * 0.125, sin2 * 0.125, cos2, sin2]).astype(np.float32)
    consts = np.zeros((128, 768), np.float32)
    a = np.arange(128)
    consts[:, 0:128] = np.eye(128)
    consts[:, 128:256] = (a[:, None] <= a[None, :])
    consts[:, 256:384] = (a[:, None] >= a[None, :])
    consts[:, 384:512] = np.where(a[None, :] >= a[:, None], 0.0, NEG)
    consts[:, 512:640] = np.where(a[None, :] <= a[:, None], 0.0, NEG)
    consts[:, 640:768] = 1.0
    return tabs, consts


def _perm_cols():
    def partner(d):
        return d + 16 if (d % 32) < 16 else d - 16
    qa = np.arange(512)
    qa_p = np.array([(j // 64) * 64 + partner(j % 64) for j in range(512)])
    ka = 512 + np.arange(128)
    ka_p = 512 + np.array([(j // 64) * 64 + partner(j % 64) for j in range(128)])
    kd0 = np.concatenate([ka[0:64], ka[0:64]])
    kd1 = np.concatenate([ka[64:128], ka[64:128]])
    kd0p = np.concatenate([ka_p[0:64], ka_p[0:64]])
    kd1p = np.concatenate([ka_p[64:128], ka_p[64:128]])
    rest = np.concatenate([np.arange(768, 1280), np.arange(640, 768), np.arange(1280, 2320)])
    return np.concatenate([qa, qa_p, kd0, kd1, kd0p, kd1p, rest])


_CACHE = {}


def prepare_inputs(cfg, inp, ncores):
    L = cfg.depth
    tabs, consts = _host_consts(cfg.S)
    cols = _perm_cols()
    w_in = np.ascontiguousarray(np.asarray(inp["w_in"], np.float32)[:, :, cols])
    rows = np.zeros((L, RW), np.float32)
    rows[:, 0:16] = inp["b_gates"]
    rows[:, 16:24] = inp["attn_sink"]
    rows[:, 24:536] = inp["g_att"]
    rows[:, 536:1048] = inp["g_ml"]
    for l in range(L):
        if l % 2 == 1:
            rows[l, 1048:1056] = inp["b_router"][l // 2]
    nm1 = max(cfg.nm, 1)
    if cfg.nm > 0:
        wr = np.ascontiguousarray(np.asarray(inp["w_router"], np.float32).reshape(cfg.nm, 8, 128, NEXP).transpose(0, 2, 1, 3))
        ew1, ew3, ew2 = inp["exp_w1"], inp["exp_w3"], inp["exp_w2"]
    else:
        wr = np.zeros((1, 128, 8, NEXP), np.float32)
        ew1 = np.zeros((1, NEXP, D, FFN), np.float32)
        ew3 = ew1
        ew2 = np.zeros((1, NEXP, FFN, D), np.float32)
    maps = []
    for b in range(ncores):
        vecs = np.zeros((128, cfg.NV), np.float32)
        vecs[:, 0:8] = _fm(inp["c"][b])
        vecs[:, 8:16] = _fm(inp["c_ctx"])
        vecs[:, 16:24] = _fm(inp["final_g"])
        for l in range(L):
            vb = 24 + 88 * l
            vecs[:, vb:vb + 8] = _fm(inp["norm1_g"][l])
            vecs[:, vb + 8:vb + 16] = _fm(inp["norm2_g"][l])
            vecs[:, vb + 16:vb + 64] = _fm(inp["b_mod"][l])
            cw = np.asarray(inp["conv_w"][l], np.float32)
            for j in range(5):
                vecs[:, vb + 64 + j * 4:vb + 68 + j * 4] = _fm(cw[j])
            vecs[:, vb + 84:vb + 88] = _fm(inp["conv_b"][l])
        maps.append({
            "x": np.ascontiguousarray(inp["x"][b], dtype=np.float32), "ctx": np.ascontiguousarray(inp["ctx"][b], dtype=np.float32),
            "vecs": vecs, "rows": rows, "w_mod": np.asarray(inp["w_mod"], np.float32), "w_in": w_in,
            "w_out": np.asarray(inp["w_out"], np.float32),
            "ffn_w1": np.asarray(inp["ffn_w1"], np.float32), "ffn_w3": np.asarray(inp["ffn_w3"], np.float32),
            "ffn_w2": np.asarray(inp["ffn_w2"], np.float32), "w_router": wr,
            "exp_w1": np.asarray(ew1, np.float32), "exp_w3": np.asarray(ew3, np.float32), "exp_w2": np.asarray(ew2, np.float32),
            "tabs": tabs, "consts": consts,
        })
    return maps


def run(cfg, inp, ncores, trace=False):
    key = (cfg.S, cfg.depth)
    if key not in _CACHE:
        _CACHE[key] = build(cfg)
    nc = _CACHE[key]
    maps = prepare_inputs(cfg, inp, ncores)
    res = run_bass_kernel_spmd(nc, maps, core_ids=list(range(ncores)), trace=trace)
    out = np.stack([res.results[b]["out"] for b in range(ncores)], 0)
    return out, res


def kernel(**inputs):
    cfg = Cfg(S=4096, depth=4)
    out, _ = run(cfg, inputs, 8)
    return out.astype(np.float32)
```
